# Optimizing a Trainium2 kernel written in Bass

```python
import jax, jax.numpy as jnp
from jax import lax
import numpy as np

D_MODEL = 1024
BATCH = 2
SEQ = 8192
DEPTH = 1

CTX_LEN = 256
GRID_W = 64

GLA_HEADS = 4
GLA_DK = 128
GLA_DV = 256
GLA_RANK = 16
GLA_TAU = 16.0
GLA_CHUNK = 64
GLA_QK = GLA_HEADS * GLA_DK
GLA_V = GLA_HEADS * GLA_DV

RET_HEADS = 4
RET_DK = 128
RET_DV = 256
RET_CHUNK = 128
RET_QK = RET_HEADS * RET_DK
RET_V = RET_HEADS * RET_DV
ROPE_BASE = 10000.0

N_GROUPS = 4
EXPERTS_PER_GROUP = 8
N_EXPERTS = N_GROUPS * EXPERTS_PER_GROUP
TOP_K = 2
EXPERT_FF = 256
MOE_BLOCK = 128

NORM_EPS = 1e-6

IN_SPLITS = (GLA_QK, GLA_QK, GLA_V, GLA_V, GLA_RANK, GLA_RANK,
             RET_QK, RET_QK, RET_V, RET_V, D_MODEL, D_MODEL)
IN_WIDTH = sum(IN_SPLITS)

kernel_name = 'hybrid_gla_retention_hmoe_dit_block'


def rmsnorm(x, g):
    xf = x.astype(jnp.float32)
    y = xf * lax.rsqrt(jnp.mean(xf * xf, axis=-1, keepdims=True) + NORM_EPS)
    return (y * g.astype(jnp.float32)).astype(x.dtype)


def head_groupnorm(o):
    of = o.astype(jnp.float32)
    mu = jnp.mean(of, axis=-1, keepdims=True)
    var = jnp.mean(jnp.square(of - mu), axis=-1, keepdims=True)
    return ((of - mu) * lax.rsqrt(var + NORM_EPS)).astype(o.dtype)


def modulate(h, shift, scale):
    return h * (1.0 + scale) + shift


def split_columns(proj):
    out = []
    start = 0
    for w in IN_SPLITS:
        out.append(proj[..., start:start + w])
        start += w
    return out


def retention_log_decays():
    h = jnp.arange(RET_HEADS, dtype=jnp.float32)
    fwd = jnp.log1p(-jnp.exp2(-5.0 - h))
    bwd = jnp.log1p(-jnp.exp2(-5.5 - h))
    return fwd, bwd


def axial_rope(t, rows, cols):
    half = t.shape[-1] // 2
    n = half // 2
    inv = ROPE_BASE ** (-jnp.arange(n, dtype=jnp.float32) / n)

    def rot(u, p):
        ang = p[:, None] * inv[None, :]
        cos = jnp.cos(ang)[None, :, None, :].astype(u.dtype)
        sin = jnp.sin(ang)[None, :, None, :].astype(u.dtype)
        u1, u2 = u[..., :n], u[..., n:]
        return jnp.concatenate([u1 * cos - u2 * sin, u1 * sin + u2 * cos], axis=-1)

    return jnp.concatenate([rot(t[..., :half], rows), rot(t[..., half:], cols)], axis=-1)


def to_chunks(t, chunk):
    B, L, H, W = t.shape
    return jnp.moveaxis(t.reshape(B, L // chunk, chunk, H, W), 1, 0)


def from_chunks(o):
    N, B, C, H, W = o.shape
    return jnp.moveaxis(o, 0, 1).reshape(B, N * C, H, W)


def gla_scan(q, k, v, log_a, s0, strict):
    C = GLA_CHUNK
    ti = jnp.arange(C)
    mask = (ti[:, None] > ti[None, :]) if strict else (ti[:, None] >= ti[None, :])
    mask5 = mask[None, :, :, None, None]

    def step(S, inp):
        qc, kc, vc, lac = inp
        b = jnp.cumsum(lac.astype(jnp.float32), axis=1)
        inter = jnp.einsum('bthk,bhkv->bthv', qc * jnp.exp(b), S)
        decay = jnp.exp(jnp.where(mask5, b[:, :, None] - b[:, None, :], -jnp.inf))
        scores = jnp.einsum('bthk,btshk,bshk->btsh', qc, decay, kc)
        intra = jnp.einsum('btsh,bshv->bthv', scores, vc)
        b_last = b[:, -1]
        S_new = jnp.exp(b_last)[..., None] * S + jnp.einsum(
            'bshk,bshv->bhkv', kc * jnp.exp(b_last[:, None] - b), vc)
        return S_new, inter + intra

    S_fin, o = lax.scan(step, s0, (to_chunks(q, C), to_chunks(k, C), to_chunks(v, C), to_chunks(log_a, C)))
    return from_chunks(o).astype(v.dtype), S_fin


def retention_scan(q, k, v, log_gamma, s0, strict):
    C = RET_CHUNK
    pos = jnp.arange(C, dtype=jnp.float32)
    ti = jnp.arange(C)
    mask = (ti[:, None] > ti[None, :]) if strict else (ti[:, None] >= ti[None, :])
    rel = pos[:, None] - pos[None, :]
    dmat = jnp.exp(jnp.where(mask[None], rel[None] * log_gamma[:, None, None], -jnp.inf))
    q_decay = jnp.exp((pos + 1.0)[:, None] * log_gamma[None, :])
    k_decay = jnp.exp((C - 1.0 - pos)[:, None] * log_gamma[None, :])
    c_decay = jnp.exp(C * log_gamma)

    def step(S, inp):
        qc, kc, vc = inp
        inter = jnp.einsum('bthk,bhkv->bthv', qc, S) * q_decay[None, :, :, None]
        scores = jnp.einsum('bthk,bshk->bhts', qc, kc) * dmat[None]
        intra = jnp.einsum('bhts,bshv->bthv', scores, vc)
        S_new = c_decay[None, :, None, None] * S + jnp.einsum(
            'bshk,bshv->bhkv', kc * k_decay[None, :, :, None], vc)
        return S_new, inter + intra

    S_fin, o = lax.scan(step, s0, (to_chunks(q, C), to_chunks(k, C), to_chunks(v, C)))
    return from_chunks(o).astype(v.dtype), S_fin


def mixer_heads(h, w_in, gla_lr_w, gla_lr_b, gla_norm, states, rope):
    B, L, _ = h.shape
    (gq, gk, gv, gg, glr_f, glr_b, rq, rk, rv, rg, m_gla, m_ret) = split_columns(h @ w_in)

    def heads(t, n):
        return t.reshape(B, L, n, -1)

    def flip(t):
        return jnp.flip(t, axis=1)

    gq = heads(gq, GLA_HEADS) * GLA_DK ** -0.5
    gk = heads(gk, GLA_HEADS)
    gv = heads(gv, GLA_HEADS)

    def log_gate(lr, d):
        z = (lr @ gla_lr_w[d] + gla_lr_b[d]).astype(jnp.float32)
        return heads(jax.nn.log_sigmoid(z) / GLA_TAU, GLA_HEADS)

    o_f, s_gf = gla_scan(gq, gk, gv, log_gate(glr_f, 0), states[0], False)
    o_b, s_gb = gla_scan(flip(gq), flip(gk), flip(gv), flip(log_gate(glr_b, 1)), states[1], True)
    o_gla = rmsnorm(o_f + flip(o_b), gla_norm) * jax.nn.silu(heads(gg, GLA_HEADS))
    o_gla = o_gla.reshape(B, L, GLA_V)

    rq = heads(rq, RET_HEADS)
    rk = heads(rk, RET_HEADS) * RET_DK ** -0.5
    rv = heads(rv, RET_HEADS)
    if rope is not None:
        rq = axial_rope(rq, rope[0], rope[1])
        rk = axial_rope(rk, rope[0], rope[1])
    lg_f, lg_b = retention_log_decays()
    r_f, s_rf = retention_scan(rq, rk, rv, lg_f, states[2], False)
    r_b, s_rb = retention_scan(flip(rq), flip(rk), flip(rv), lg_b, states[3], True)
    o_ret = head_groupnorm(r_f + flip(r_b)) * jax.nn.silu(heads(rg, RET_HEADS))
    o_ret = o_ret.reshape(B, L, RET_V)
    return o_gla, o_ret, m_gla, m_ret, (s_gf, s_gb, s_rf, s_rb)


def merge_branches(o_gla, o_ret, m_gla, m_ret, w_branch_gla, w_branch_ret, w_out):
    y = jax.nn.sigmoid(m_gla) * (o_gla @ w_branch_gla) + jax.nn.sigmoid(m_ret) * (o_ret @ w_branch_ret)
    return y @ w_out


def hier_moe(h, w_rg, b_rg, w_re, b_re, w_gate, w_up, w_down):
    B, L, D = h.shape
    T = B * L
    xt = h.reshape(T, D)
    g_logits = (xt @ w_rg + b_rg).astype(jnp.float32)
    g_prob = jax.nn.softmax(g_logits, axis=-1)
    g_sel = jnp.argmax(g_logits, axis=-1).astype(jnp.int32)
    g_w = jnp.take_along_axis(g_prob, g_sel[:, None], axis=-1)[:, 0]
    e_logits = (xt @ w_re + b_re).astype(jnp.float32).reshape(T, N_GROUPS, EXPERTS_PER_GROUP)
    e_logits = jnp.take_along_axis(e_logits, g_sel[:, None, None], axis=1)[:, 0]
    top_v, top_i = lax.top_k(e_logits, TOP_K)
    e_w = jax.nn.softmax(top_v, axis=-1) * g_w[:, None]
    expert_id = g_sel[:, None] * EXPERTS_PER_GROUP + top_i.astype(jnp.int32)

    A = T * TOP_K
    flat_e = expert_id.reshape(A)
    flat_tok = jnp.repeat(jnp.arange(T, dtype=jnp.int32), TOP_K)
    flat_w = e_w.reshape(A)
    order = jnp.argsort(flat_e)
    se, stok, sw = flat_e[order], flat_tok[order], flat_w[order]
    counts = jax.ops.segment_sum(jnp.ones((A,), jnp.int32), flat_e, num_segments=N_EXPERTS)
    starts = jnp.cumsum(counts) - counts
    padded = (counts + MOE_BLOCK - 1) // MOE_BLOCK * MOE_BLOCK
    pends = jnp.cumsum(padded)
    pstarts = pends - padded
    dest = pstarts[se] + jnp.arange(A, dtype=jnp.int32) - starts[se]
    P = A + N_EXPERTS * MOE_BLOCK
    NB = P // MOE_BLOCK
    x_pad = jnp.zeros((P, D), h.dtype).at[dest].set(xt[stok])
    tok_pad = jnp.zeros((P,), jnp.int32).at[dest].set(stok)
    w_pad = jnp.zeros((P,), jnp.float32).at[dest].set(sw)
    block_start = jnp.arange(NB, dtype=jnp.int32) * MOE_BLOCK
    block_e = jnp.minimum(jnp.searchsorted(pends, block_start, side='right'), N_EXPERTS - 1)

    def expert_block(args):
        xb, e = args
        hid = jax.nn.silu(xb @ w_gate[e]) * (xb @ w_up[e])
        return hid @ w_down[e]

    y = lax.map(expert_block, (x_pad.reshape(NB, MOE_BLOCK, D), block_e)).reshape(P, D)
    out = jnp.zeros((T, D), jnp.float32).at[tok_pad].add(y.astype(jnp.float32) * w_pad[:, None])
    return out.astype(h.dtype).reshape(B, L, D)


def setup_inputs(seed: int = 0) -> dict:
    key = jax.random.key(seed)
    ks = jax.random.split(key, 24)

    def nrm(k, shape, scale):
        return jax.random.normal(k, shape, jnp.float32) * scale

    D = D_MODEL
    return {
        'x': nrm(ks[0], (BATCH, SEQ, D), 1.0),
        'c': nrm(ks[1], (BATCH, D), 1.0),
        'ctx': nrm(ks[2], (BATCH, CTX_LEN, D), 1.0),
        'c_ctx': nrm(ks[3], (D,), 1.0),
        'w_ada': nrm(ks[4], (DEPTH, D, 6 * D), D ** -0.5),
        'b_ada': nrm(ks[5], (DEPTH, 6 * D), 0.02),
        'norm_mix': 1.0 + nrm(ks[6], (DEPTH, D), 0.05),
        'norm_ffn': 1.0 + nrm(ks[7], (DEPTH, D), 0.05),
        'w_in': nrm(ks[8], (DEPTH, D, IN_WIDTH), D ** -0.5),
        'gla_lr_w': nrm(ks[9], (DEPTH, 2, GLA_RANK, GLA_QK), GLA_RANK ** -0.5),
        'gla_lr_b': nrm(ks[10], (DEPTH, 2, GLA_QK), 0.1),
        'gla_norm': 1.0 + nrm(ks[11], (DEPTH, GLA_DV), 0.05),
        'w_branch_gla': nrm(ks[12], (DEPTH, GLA_V, D), GLA_V ** -0.5),
        'w_branch_ret': nrm(ks[13], (DEPTH, RET_V, D), RET_V ** -0.5),
        'w_out': nrm(ks[14], (DEPTH, D, D), D ** -0.5),
        'w_router_group': nrm(ks[15], (DEPTH, D, N_GROUPS), D ** -0.5),
        'b_router_group': nrm(ks[16], (DEPTH, N_GROUPS), 0.01),
        'w_router_expert': nrm(ks[17], (DEPTH, D, N_EXPERTS), D ** -0.5),
        'b_router_expert': nrm(ks[18], (DEPTH, N_EXPERTS), 0.01),
        'w_expert_gate': nrm(ks[19], (DEPTH, N_EXPERTS, D, EXPERT_FF), D ** -0.5),
        'w_expert_up': nrm(ks[20], (DEPTH, N_EXPERTS, D, EXPERT_FF), D ** -0.5),
        'w_expert_down': nrm(ks[21], (DEPTH, N_EXPERTS, EXPERT_FF, D), EXPERT_FF ** -0.5),
        'norm_final': 1.0 + nrm(ks[22], (D,), 0.05),
    }


def reference(x, c, ctx, c_ctx, w_ada, b_ada, norm_mix, norm_ffn, w_in, gla_lr_w, gla_lr_b, gla_norm,
              w_branch_gla, w_branch_ret, w_out, w_router_group, b_router_group, w_router_expert,
              b_router_expert, w_expert_gate, w_expert_up, w_expert_down, norm_final):
    B, L, _ = x.shape
    ROWS = L // GRID_W
    rows = jnp.repeat(jnp.arange(ROWS, dtype=jnp.float32), GRID_W)
    cols = jnp.tile(jnp.arange(GRID_W, dtype=jnp.float32), ROWS)
    zero_states = (
        jnp.zeros((B, GLA_HEADS, GLA_DK, GLA_DV), jnp.float32),
        jnp.zeros((B, GLA_HEADS, GLA_DK, GLA_DV), jnp.float32),
        jnp.zeros((B, RET_HEADS, RET_DK, RET_DV), jnp.float32),
        jnp.zeros((B, RET_HEADS, RET_DK, RET_DV), jnp.float32),
    )
    h_lat = x
    h_ctx = ctx
    for layer in range(DEPTH):
        sh1, sc1, g1, sh2, sc2, g2 = jnp.split(
            (jax.nn.silu(c) @ w_ada[layer] + b_ada[layer])[:, None, :], 6, axis=-1)
        csh1, csc1, cg1, csh2, csc2, cg2 = jnp.split(
            (jax.nn.silu(c_ctx) @ w_ada[layer] + b_ada[layer])[None, None, :], 6, axis=-1)
        mix_args = (w_in[layer], gla_lr_w[layer], gla_lr_b[layer], gla_norm[layer])
        merge_args = (w_branch_gla[layer], w_branch_ret[layer], w_out[layer])
        moe_args = (w_router_group[layer], b_router_group[layer], w_router_expert[layer],
                    b_router_expert[layer], w_expert_gate[layer], w_expert_up[layer], w_expert_down[layer])

        hc = modulate(rmsnorm(h_ctx, norm_mix[layer]), csh1, csc1)
        c_gla, c_ret, c_mg, c_mr, ctx_states = mixer_heads(hc, *mix_args, zero_states, None)

        hl = modulate(rmsnorm(h_lat, norm_mix[layer]), sh1, sc1)
        l_gla, l_ret, l_mg, l_mr, _ = mixer_heads(hl, *mix_args, ctx_states, (rows, cols))
        h_lat = h_lat + g1 * merge_branches(l_gla, l_ret, l_mg, l_mr, *merge_args)
        h_lat = h_lat + g2 * hier_moe(modulate(rmsnorm(h_lat, norm_ffn[layer]), sh2, sc2), *moe_args)

        if layer < DEPTH - 1:
            h_ctx = h_ctx + cg1 * merge_branches(c_gla, c_ret, c_mg, c_mr, *merge_args)
            h_ctx = h_ctx + cg2 * hier_moe(modulate(rmsnorm(h_ctx, norm_ffn[layer]), csh2, csc2), *moe_args)
    return rmsnorm(h_lat, norm_final)
```

```python
import contextlib
import numpy as np
import concourse.bass as bass
import concourse.mybir as mybir
from concourse.bass_utils import run_bass_kernel_spmd

F32 = mybir.dt.float32
BF16 = mybir.dt.bfloat16
AF = mybir.ActivationFunctionType
ALU = mybir.AluOpType
AX = mybir.AxisListType

D = 1024
SEG = 2048
NT = 16
CTX = 256
NCT = 2
GQ, GK, GV, GG, LRF, LRB, RQ, RK, RV, RG, MG, MR = 0, 512, 1024, 2048, 3072, 3088, 3104, 3616, 4128, 5152, 6176, 7200
EPS = 1e-6
TAU = 16.0
BIG = 30000.0

C_ID, C_LE, C_GT, C_GE, C_LT, C_ONE = 0, 128, 256, 384, 512, 640
C_RF = 768
C_RD = 792
C_RDT = 800
C_CM = 808
C_EPS = 816
C_CS = 832
NCS = 832
NCST = C_CS + NT * 256
NCB = 768

COMPUTE = ("pe", "act", "dve", "pool")
import re as _re
_PSUM_RE = _re.compile(r"^(B\d|Bt|pa|pg\d|PM\d|PB\d|PO\d|PT|PG\d|PD\d|PX|PF)$")


class Prog:
    NDMA = 32

    def __init__(self, nc, es, needed=None):
        self.nc = nc
        self.learn = needed is None
        self.needed_in = needed or set()
        self.needed_out = set()
        self.eng = {"pe": nc.tensor, "act": nc.scalar, "dve": nc.vector, "pool": nc.gpsimd, "sp": nc.sync}
        self.sem = {e: es.enter_context(nc.semaphore("s_" + e)) for e in COMPUTE}
        self.dsem = [es.enter_context(nc.semaphore("s_dma%d" % i)) for i in range(self.NDMA)]
        self.qn = {"sp": 0, "pool": 0, "act": 0}
        self.csem = es.enter_context(nc.semaphore("s_cc"))
        self.dcnt = [0] * self.NDMA
        self.dlast = [None] * self.NDMA
        self.ccnt = 0
        self.nd = 0
        self.seq = {e: 0 for e in COMPUTE}
        self.ops = []
        self.lastw = {}
        self.readers = {}
        self.waited = {e: {} for e in self.eng}
        self.last_on = {e: None for e in self.eng}
        self.nwait = 0
        self.kn = {e: {} for e in self.eng}
        self.clk = []

    def _merge(self, eng, opid):
        k = self.kn[eng]
        for e, v in self.clk[opid].items():
            if k.get(e, -1) < v:
                k[e] = v

    def _wait(self, eng, opid):
        kind, sem, val, oeng = self.ops[opid]
        if kind == "c" and self.kn[eng].get(oeng, -1) >= opid:
            return
        if val is None:
            return
        w = self.waited[eng]
        key = id(sem)
        if w.get(key, 0) >= val:
            self._merge(eng, opid)
            return
        w[key] = val
        self.needed_out.add(opid)
        self.eng[eng].wait_ge(sem, val)
        self.nwait += 1
        self._merge(eng, opid)

    def _deps(self, reads, writes):
        deps = set()
        for b in reads:
            if b in self.lastw:
                deps.add(self.lastw[b])
        for b in writes:
            if b in self.lastw:
                deps.add(self.lastw[b])
            for r in self.readers.get(b, ()):
                deps.add(r)
        return deps

    def _commit(self, opid, reads, writes):
        for b in reads:
            self.readers.setdefault(b, []).append(opid)
        for b in writes:
            self.lastw[b] = opid
            self.readers[b] = []

    def op(self, eng, fn, reads=(), writes=()):
        pr = [b for b in reads if _PSUM_RE.match(b)]
        if pr:
            writes = list(writes) + [b for b in pr if b not in writes]
            reads = [b for b in reads if b not in pr]
        opid = len(self.ops)
        raw = {self.lastw[b] for b in reads if b in self.lastw}
        for d in sorted(self._deps(reads, writes)):
            k, s, v, oe = self.ops[d]
            if oe == eng and k == "c" and (eng == "pe" or d not in raw):
                continue
            self._wait(eng, d)
        ins = fn()
        if self.learn or (opid in self.needed_in):
            self.seq[eng] += 1
            ins.then_inc(self.sem[eng], 1)
            self.ops.append(("c", self.sem[eng], self.seq[eng], eng))
        else:
            self.ops.append(("c", self.sem[eng], None, eng))
        c = dict(self.kn[eng])
        c[eng] = opid
        self.clk.append(c)
        self._commit(opid, reads, writes)
        self.last_on[eng] = opid
        return ins

    def dma(self, q, out, in_, reads=(), writes=(), **kw):
        opid = len(self.ops)
        for d in sorted(self._deps(reads, writes)):
            self._wait(q, d)
        half = self.NDMA // 2
        if q == "pool":
            i = half + self.qn[q] % half
        else:
            i = self.qn[q] % half
        self.qn[q] += 1
        self.nd += 1
        if self.dlast[i] is not None:
            self._wait(q, self.dlast[i])
        ins = self.eng[q].dma_start(out=out, in_=in_, **kw)
        self.dcnt[i] += 16
        ins.then_inc(self.dsem[i], 16)
        self.ops.append(("d", self.dsem[i], self.dcnt[i], q))
        self.clk.append(dict(self.kn[q]))
        self.dlast[i] = opid
        self._commit(opid, reads, writes)
        return ins

    def collective(self, kind, alu, groups, ins_, outs_, reads=(), writes=()):
        opid = len(self.ops)
        for d in sorted(self._deps(reads, writes)):
            self._wait("pool", d)
        ins = self.nc.gpsimd.collective_compute(kind, alu, replica_groups=groups, ins=ins_, outs=outs_)
        self.ccnt += 1
        ins.then_inc(self.csem, 1)
        self.ops.append(("x", self.csem, self.ccnt, "pool"))
        self.clk.append(dict(self.kn["pool"]))
        self._commit(opid, reads, writes)
        return ins

    def barrier(self):
        lasts = [self.last_on[e] for e in COMPUTE if self.last_on[e] is not None]
        lasts += [d for d in self.dlast if d is not None]
        for e in self.eng:
            for d in lasts:
                k, s, v, oe = self.ops[d]
                if oe == e and k == "c":
                    continue
                self._wait(e, d)
        self.lastw.clear()
        self.readers.clear()

    def wait_all(self, eng):
        for d in self.dlast:
            if d is not None:
                self._wait(eng, d)
        for e in COMPUTE:
            if self.last_on[e] is not None and e != eng:
                self._wait(eng, self.last_on[e])


class _Stop(Exception):
    pass


def build_program(needed=None, dbg=None, stop=None):
    try:
        return _build(needed, dbg, stop)
    except _Stop as e:
        return e.args


def _build(needed=None, dbg=None, stop=None):
    nc = bass.Bass("TRN2", target_bir_lowering=False)

    def din(name, shape, dt=F32):
        return nc.dram_tensor(name, list(shape), dt, kind="ExternalInput").ap()

    x_d = din("x", [SEG, D])
    ctx_d = din("ctx", [CTX, D])
    cst_d = din("cst", [128, NCST])
    vec_d = din("vec", [128, 72])
    rowv_d = din("rowv", [4, D])
    w_ada_d = din("w_ada", [D, 6 * D])
    w_in_d = din("w_in", [D, 8224])
    lrw_d = din("lrw", [17, 2, 512])
    wbg_d = din("w_branch_gla", [D, D])
    wbr_d = din("w_branch_ret", [D, D])
    wout_d = din("w_out", [D, D])
    wr_d = din("w_router", [D, 36])
    br_d = din("b_router", [1, 36])
    if stop is None or stop == 7:
        weg_d = din("w_expert_gate", [32, D, 256])
        weu_d = din("w_expert_up", [32, D, 256])
        wed_d = din("w_expert_down", [32, 256, D])
    y_d = nc.dram_tensor("y", [SEG, D], F32, kind="ExternalOutput").ap()
    dbg_out = {}
    if dbg:
        for k, shp in dbg.items():
            dbg_out[k] = nc.dram_tensor("dbg_" + k, list(shp), F32, kind="ExternalOutput").ap()

    ogT_d = nc.dram_tensor("ogT_scratch", [16 * 128, SEG], BF16)
    cin_d = [nc.dram_tensor("cc_in%d" % u, [128, 520], F32) for u in range(8)]
    cout_d = [nc.dram_tensor("cc_out%d" % u, [4 * 128, 520], F32) for u in range(8)]

    with contextlib.ExitStack() as es:
        P = Prog(nc, es, needed)
        V = nc.vector
        A = nc.scalar
        T = nc.tensor

        def sb(st, name, shape, dt=F32):
            return st.enter_context(nc.sbuf_tensor("sb_" + name, list(shape), dt))

        def ck(k):
            if stop == k:
                P.wait_all("sp")
                raise _Stop(nc, P)

        def pbank(st, name, dt=F32):
            return st.enter_context(nc.psum_tensor("ps_" + name, [128, 512 if dt == F32 else 1024], dt))

        cst = sb(es, "cst", [128, NCS])
        cstb = sb(es, "cstb", [128, NCB], BF16)
        vec = sb(es, "vec", [128, 72])
        mod = sb(es, "mod", [128, 48])
        g1bc = sb(es, "g1bc", [128, D])
        g2bc = sb(es, "g2bc", [128, D])
        nfbc = sb(es, "nfbc", [128, D])
        P.dma("sp", cst[:], cst_d[:, 0:NCS], writes=["cst"])
        P.dma("pool", cstb[:], cst_d[:, 0:NCB], writes=["cstb"])
        P.dma("sp", vec[:], vec_d, writes=["vec"])
        P.dma("sp", g1bc[:], rowv_d[0:1, :].partition_broadcast(128), writes=["g1bc"])
        P.dma("sp", g2bc[:], rowv_d[1:2, :].partition_broadcast(128), writes=["g2bc"])
        P.dma("sp", nfbc[:], rowv_d[2:3, :].partition_broadcast(128), writes=["nfbc"])
        ident_f = cst[:, C_ID:C_ID + 128]
        ident_b = cstb[:, C_ID:C_ID + 128]
        eps_ap = cst[:, C_EPS:C_EPS + 1]
        lnqs_ap = cst[:, C_EPS + 1:C_EPS + 2]
        VC_C, VC_CC, VC_BSH1, VC_BSC1, VC_BSH2, VC_BSC2, VC_NM, VC_NF, VC_GN = 0, 8, 16, 24, 32, 40, 48, 56, 64
        M_GM1, M_SH1, M_CGM1, M_CSH1, M_GM2, M_SH2 = 0, 8, 16, 24, 32, 40

        with contextlib.ExitStack() as s0:
            wa = [sb(s0, "wa%d" % i, [128, 8, 512]) for i in range(2)]
            sil = sb(s0, "sil", [128, 16])
            silbc = sb(s0, "silbc", [128, 8, 128])
            pa = pbank(s0, "pa")
            pg = [pbank(s0, "pg%d" % i) for i in range(2)]
            P.op("act", lambda: A.activation(out=sil[:], in_=vec[:, 0:16], func=AF.Silu), reads=["vec"], writes=["sil"])
            P.op("dve", lambda: V.tensor_copy(out=silbc[:], in_=sil[:, 0:8].unsqueeze(2).to_broadcast([128, 8, 128])),
                 reads=["sil"], writes=["silbc"])
            gi = 0
            for grp in (0, 1, 3, 4, 2, 5):
                for half in range(2):
                    buf = wa[gi % 2]
                    key = "wa%d" % (gi % 2)
                    gi += 1
                    c0 = grp * 1024 + half * 512
                    P.dma("sp", buf[:], w_ada_d[:, c0:c0 + 512].rearrange("(kc p) c -> p kc c", p=128), writes=[key])
                    if grp in (0, 1, 3, 4):
                        for cc in range(4):
                            ch = half * 4 + cc
                            col = grp * 16 + ch * 2
                            for kc in range(8):
                                P.op("pe", lambda cc=cc, kc=kc, col=col, buf=buf: T.matmul(
                                    pa[:, col:col + 2], buf[:, kc, cc * 128:(cc + 1) * 128],
                                    sil[:, kc:kc + 9:8], start=(kc == 0), stop=(kc == 7)),
                                    reads=[key, "sil"], writes=["pa"])
                    else:
                        pgt = pg[half]
                        for kc in range(8):
                            P.op("pe", lambda kc=kc, buf=buf, pgt=pgt: T.matmul(
                                pgt[:, 0:512], silbc[:, kc, :], buf[:, kc, :], start=(kc == 0), stop=(kc == 7)),
                                reads=[key, "silbc"], writes=["pg%d" % half])
                        dst = g1bc if grp == 2 else g2bc
                        dk = "g1bc" if grp == 2 else "g2bc"
                        P.op("dve", lambda dst=dst, pgt=pgt, half=half: V.tensor_tensor(
                            out=dst[:, half * 512:(half + 1) * 512], in0=pgt[:, 0:512],
                            in1=dst[:, half * 512:(half + 1) * 512], op=ALU.add),
                            reads=["pg%d" % half, dk], writes=[dk])
            pav = pa[:, 0:96].rearrange("p (g c w) -> p g c w", g=6, c=8, w=2)
            tmpm = sb(s0, "tmpm", [128, 48])
            P.op("dve", lambda: V.tensor_tensor(out=mod[:, M_SH1:M_SH1 + 8], in0=pav[:, 0, :, 0], in1=vec[:, VC_BSH1:VC_BSH1 + 8], op=ALU.add), reads=["pa", "vec"], writes=["mod"])
            P.op("dve", lambda: V.tensor_tensor(out=mod[:, M_CSH1:M_CSH1 + 8], in0=pav[:, 0, :, 1], in1=vec[:, VC_BSH1:VC_BSH1 + 8], op=ALU.add), reads=["pa", "vec"], writes=["mod"])
            P.op("dve", lambda: V.tensor_tensor(out=mod[:, M_SH2:M_SH2 + 8], in0=pav[:, 3, :, 0], in1=vec[:, VC_BSH2:VC_BSH2 + 8], op=ALU.add), reads=["pa", "vec"], writes=["mod"])
            for (dstc, grp, w, bcol, ncol) in ((M_GM1, 1, 0, VC_BSC1, VC_NM), (M_CGM1, 1, 1, VC_BSC1, VC_NM), (M_GM2, 4, 0, VC_BSC2, VC_NF)):
                P.op("dve", lambda grp=grp, w=w, bcol=bcol: V.scalar_tensor_tensor(
                    out=tmpm[:, 0:8], in0=pav[:, grp, :, w], scalar=1.0, in1=vec[:, bcol:bcol + 8], op0=ALU.add, op1=ALU.add),
                    reads=["pa", "vec"], writes=["tmpm"])
                P.op("dve", lambda dstc=dstc, ncol=ncol: V.tensor_tensor(
                    out=mod[:, dstc:dstc + 8], in0=tmpm[:, 0:8], in1=vec[:, ncol:ncol + 8], op=ALU.mult),
                    reads=["tmpm", "vec"], writes=["mod"])
        P.barrier()
        ck(0)

        def norm_T(xt, xkey, ptr, ptrkey, dst_fn, dstkey, gmc, shc, sc):
            sq, ss, xs = sc["sq"], sc["ss"], sc["xs"]
            P.op("act", lambda: A.activation(out=sq, in_=xt, func=AF.Square, accum_out=ss[:, 0:1]),
                 reads=[xkey], writes=["n_sq", "n_ss"])
            P.op("act", lambda: A.activation(out=ss[:, 1:2], in_=ss[:, 0:1], func=AF.Sqrt, bias=eps_ap, scale=1.0 / D),
                 reads=["n_ss", "cst"], writes=["n_ss1"])
            P.op("dve", lambda: V.reciprocal(out=ss[:, 2:3], in_=ss[:, 1:2]), reads=["n_ss1"], writes=["n_ss2"])
            P.op("dve", lambda: V.tensor_scalar(out=xs, in0=xt, scalar1=ss[:, 2:3], scalar2=None, op0=ALU.mult),
                 reads=[xkey, "n_ss2"], writes=["n_xs"])
            for kc in range(8):
                P.op("pe", lambda kc=kc: T.transpose(ptr[:, kc * 128:(kc + 1) * 128], xs[:, kc * 128:(kc + 1) * 128], ident_b),
                     reads=["n_xs", "cstb"], writes=[ptrkey])
            for kc in range(8):
                if kc % 2 == 0:
                    P.op("act", lambda kc=kc: A.activation(out=dst_fn(kc), in_=ptr[:, kc * 128:(kc + 1) * 128], func=AF.Identity,
                                                           scale=mod[:, gmc + kc:gmc + kc + 1], bias=mod[:, shc + kc:shc + kc + 1]),
                         reads=[ptrkey, "mod"], writes=[dstkey])
                else:
                    P.op("dve", lambda kc=kc: V.tensor_scalar(out=dst_fn(kc), in0=ptr[:, kc * 128:(kc + 1) * 128],
                                                              scalar1=mod[:, gmc + kc:gmc + kc + 1], scalar2=mod[:, shc + kc:shc + kc + 1],
                                                              op0=ALU.mult, op1=ALU.add),
                         reads=[ptrkey, "mod"], writes=[dstkey])

        with contextlib.ExitStack() as s2:
            hT = sb(s2, "hT", [128, 8, SEG], BF16)
            hcT = sb(s2, "hcT", [128, 8, CTX], BF16)
            lrT = [sb(s2, "lrT%d" % d, [17, SEG + CTX], BF16) for d in range(2)]
            lrw = sb(s2, "lrw", [17, 2, 512], BF16)
            wlr = sb(s2, "wlr", [128, 8, 32], BF16)
            QT = [sb(s2, "QT%d" % s, [128, NT, 2, 128], BF16) for s in range(2)]
            AT = [sb(s2, "AT%d" % s, [128, NT, 128], BF16) for s in range(2)]
            KH = [sb(s2, "KH%d" % s, [128, NT + NCT, 2, 128], BF16) for s in range(2)]
            VV = [sb(s2, "VV%d" % s, [128, NT + NCT, 256], BF16) for s in range(2)]
            DFB = [sb(s2, "DFB%d" % s, [128, NT + NCT, 2]) for s in range(2)]
            TOT = [sb(s2, "TOT%d" % s, [128, NT, 2]) for s in range(2)]
            XP = [sb(s2, "XP", [128, 520])] * 2
            XG = [sb(s2, "XG", [128, 4, 520])] * 2
            SC = [sb(s2, "SC", [128, 2, 256])] * 2
            CS = [sb(s2, "CS%d" % i, [128, 256]) for i in range(2)]
            ST = sb(s2, "ST", [128, 2, 256])
            STb = [sb(s2, "STb%d" % i, [128, 256], BF16) for i in range(2)]
            GS = sb(s2, "GS", [128, NT, 256], BF16)
            OGT = [sb(s2, "OGT", [128, 2, SEG], BF16)] * 2
            WQKV = [sb(s2, "WQKV%d" % s, [128, 8, 512], BF16) for s in range(2)]
            WGt = [sb(s2, "WG%d" % s, [128, 8, 256], BF16) for s in range(2)]
            esp = sb(s2, "esp", [128, 256])
            sp_hi = sb(s2, "sp_hi", [128, 256], BF16)
            sp_lo = sb(s2, "sp_lo", [128, 256], BF16)
            FA = [sb(s2, "FA%d" % i, [128, 6, 128]) for i in range(2)]
            QK4 = sb(s2, "QK4", [128, 4, 128], BF16)
            KTt = sb(s2, "KTt", [128, 2, 128], BF16)
            tAB = sb(s2, "tAB", [128, 256])
            qr = sb(s2, "qr", [128, 2, 128])
            rt1 = sb(s2, "rt1", [128, 2, 128])
            rt2 = sb(s2, "rt2", [128, 2, 128])
            sgt = [sb(s2, "sgt%d" % i, [128, 256]) for i in range(2)]
            ogt = [sb(s2, "ogt%d" % i, [128, 256], BF16) for i in range(2)]
            otmp = [sb(s2, "otmp%d" % i, [128, 256]) for i in range(2)]
            st8 = sb(s2, "st8", [128, 16])
            dpr = sb(s2, "dpr", [128, 8])
            xtmp = sb(s2, "xtmp", [128, 256])
            B = [pbank(s2, "B%d" % i) for i in range(4)]
            Bt = pbank(s2, "Bt", BF16)
            B5 = pbank(s2, "B5")
            B6 = pbank(s2, "B6")
            B7 = pbank(s2, "B7")
            print("phase2 sbuf remaining", nc.sbuf_bytes_remaining)

            xgf = XG[0][:].rearrange("p a b -> p (a b)")
            gsf = GS[:].rearrange("p a b -> p (a b)")
            xt2 = [xgf[:, 0:D], xgf[:, D:2 * D]]
            nsc = {"sq": gsf[:, 0:D], "ss": st8[:, 0:4], "xs": gsf[:, D:2 * D]}
            P.dma("pool", wlr[:], w_in_d[:, LRF:LRF + 32].rearrange("(kc p) c -> p kc c", p=128), writes=["wlr"])
            P.dma("pool", lrw[:], lrw_d, writes=["lrw"])
            for n in range(NT + NCT):
                xt = xt2[n % 2]
                xk = "xt%d" % (n % 2)
                if n < NT:
                    P.dma("sp", xt, x_d[n * 128:(n + 1) * 128, :], writes=[xk])
                    norm_T(xt, xk, Bt, "Bt", lambda kc, n=n: hT[:, kc, n * 128:(n + 1) * 128], "hT", M_GM1, M_SH1, nsc)
                else:
                    m = n - NT
                    P.dma("sp", xt, ctx_d[m * 128:(m + 1) * 128, :], writes=[xk])
                    norm_T(xt, xk, Bt, "Bt", lambda kc, m=m: hcT[:, kc, m * 128:(m + 1) * 128], "hcT", M_CGM1, M_CSH1, nsc)
            P.barrier()
            ck(1)
            for d in range(2):
                P.op("dve", lambda d=d: V.memset(lrT[d][:], 1.0), writes=["lrT%d" % d])
            for blk in range(5):
                src = hT if blk < 4 else hcT
                w = 512 if blk < 4 else CTX
                o0 = blk * 512 if blk < 4 else 0
                for d in range(2):
                    for kc in range(8):
                        P.op("pe", lambda d=d, kc=kc, src=src, o0=o0, w=w: T.matmul(
                            B[d][0:16, 0:w], wlr[:, kc, d * 16:(d + 1) * 16], src[:, kc, o0:o0 + w],
                            start=(kc == 0), stop=(kc == 7)), reads=["wlr", "hT", "hcT"], writes=["B%d" % d])
                    P.op("act", lambda d=d, blk=blk, w=w: A.copy(out=lrT[d][0:16, blk * 512:blk * 512 + w], in_=B[d][0:16, 0:w]),
                         reads=["B%d" % d], writes=["lrT%d" % d])

            ck(11)
            def load_weights(u, which):
                s = u % 2
                isg = u < 4
                h = u % 4
                qo, ko, vo, go = (GQ, GK, GV, GG) if isg else (RQ, RK, RV, RG)
                r = lambda c0, w: w_in_d[:, c0:c0 + w].rearrange("(kc p) c -> p kc c", p=128)
                if which == "qkv":
                    P.dma("pool", WQKV[s][:, :, 0:128], r(qo + h * 128, 128), writes=["WQKV%d" % s])
                    P.dma("pool", WQKV[s][:, :, 128:256], r(ko + h * 128, 128), writes=["WQKV%d" % s])
                    P.dma("pool", WQKV[s][:, :, 256:512], r(vo + h * 256, 256), writes=["WQKV%d" % s])
                else:
                    P.dma("pool", WGt[s][:], r(go + h * 256, 256), writes=["WG%d" % s])

            def passA(u):
                s = u % 2
                isg = u < 4
                h = u % 4
                qs = 128.0 ** -0.5 if isg else 1.0
                NA = NT + NCT

                def geom(n):
                    lat = n < NT
                    src = hT if lat else hcT
                    t0 = n * 128 if lat else (n - NT) * 128
                    lcol = n * 128 if lat else SEG + (n - NT) * 128
                    return lat, src, t0, lcol

                def stage1(n):
                    lat, src, t0, lcol = geom(n)
                    pp = n % 2
                    pq = B[pp]
                    pqk = "B%d" % pp
                    steps = []

                    def a0():
                        for kc in range(8):
                            P.op("pe", lambda kc=kc: T.matmul(pq[:, 0:512], src[:, kc, t0:t0 + 128], WQKV[s][:, kc, :], start=(kc == 0), stop=(kc == 7)),
                                 reads=["hT", "hcT", "WQKV%d" % s], writes=[pqk])
                        if isg:
                            for d in range(2):
                                P.op("pe", lambda d=d: T.matmul(B[2][:, d * 128:(d + 1) * 128], lrT[d][0:17, lcol:lcol + 128], lrw[0:17, d, h * 128:(h + 1) * 128],
                                                                start=True, stop=True), reads=["lrT%d" % d, "lrw"], writes=["B2"])
                        elif lat:
                            P.dma("sp", CS[pp][:], cst_d[:, C_CS + n * 256:C_CS + (n + 1) * 256], writes=["CS%d" % pp])

                    def a1():
                        P.op("act", lambda: A.copy(out=VV[s][:, n, :], in_=pq[:, 256:512]), reads=[pqk], writes=["VV%d_%d" % (s, n)])
                        if isg:
                            P.op("act", lambda: A.activation(out=esp[:], in_=B[2][:, 0:256], func=AF.Exp, scale=-1.0), reads=["B2"], writes=["esp"])
                            P.op("act", lambda: A.activation(out=esp[:], in_=esp[:], func=AF.Ln, bias=1.0), reads=["esp"], writes=["esp"])

                    def a2():
                        P.op("dve", lambda: V.tensor_copy(out=sp_hi[:], in_=esp[:]), reads=["esp"], writes=["sp_hi"])
                        P.op("dve", lambda: V.tensor_tensor(out=sp_lo[:], in0=esp[:], in1=sp_hi[:], op=ALU.subtract), reads=["esp", "sp_hi"], writes=["sp_lo"])

                    def a3():
                        for ci, (mo, d) in enumerate(((C_LE, 0), (C_GT, 0), (C_GE, 1), (C_LT, 1))):
                            for pi, part in enumerate((sp_hi, sp_lo)):
                                P.op("pe", lambda ci=ci, mo=mo, d=d, part=part, pi=pi: T.matmul(
                                    B[3][:, ci * 128:(ci + 1) * 128], cstb[:, mo:mo + 128], part[:, d * 128:(d + 1) * 128],
                                    start=(pi == 0), stop=(pi == 1)), reads=["cstb", "sp_hi", "sp_lo"], writes=["B3"])
                        for d in range(2):
                            for pi, part in enumerate((sp_hi, sp_lo)):
                                P.op("pe", lambda d=d, part=part, pi=pi: T.matmul(
                                    B[2][:, 256 + 2 * d:258 + 2 * d], part[:, d * 128:(d + 1) * 128], cstb[:, C_ONE:C_ONE + 2],
                                    start=(pi == 0), stop=(pi == 1)), reads=["cstb", "sp_hi", "sp_lo"], writes=["B2"])

                    def a4():
                        cumv = B[3][:, 0:512].rearrange("p (a b) -> p a b", a=4)
                        fa = FA[pp]
                        P.op("act", lambda: A.activation(out=fa[:, 0:3:2, :], in_=cumv[:, 0:3:2, :], func=AF.Exp, scale=-1.0 / TAU, bias=lnqs_ap),
                             reads=["B3", "cst"], writes=["FA%d" % pp])
                        P.op("act", lambda: A.activation(out=fa[:, 4:6, :], in_=cumv[:, 1:4:2, :], func=AF.Exp, scale=-1.0 / TAU),
                             reads=["B3"], writes=["FA%d" % pp])
                        P.op("act", lambda: A.activation(out=fa[:, 1:4:2, :], in_=cumv[:, 0:3:2, :], func=AF.Exp, scale=1.0 / TAU),
                             reads=["B3"], writes=["FA%d" % pp])
                        P.op("act", lambda: A.activation(out=DFB[s][:, n, :], in_=B[2][:, 256:260:2], func=AF.Exp, scale=-1.0 / TAU),
                             reads=["B2"], writes=["DFB%d" % s])
                        if lat:
                            P.op("dve", lambda: V.tensor_copy(out=TOT[s][:, n, :], in_=B[2][:, 256:260:2]), reads=["B2"], writes=["TOT%d" % s])

                    steps = [a0, a1] + ([a2, a3, a4] if isg else [])
                    return steps

                def stage2(n):
                    lat, src, t0, lcol = geom(n)
                    pp = n % 2
                    pq = B[pp]
                    pqk = "B%d" % pp
                    qps = pq[:, 0:128]
                    kps = pq[:, 128:256]
                    steps = []
                    if isg:
                        def b0():
                            fa = FA[pp]
                            qk = pq[:, 0:256].rearrange("p (j w) -> p j w", j=2)
                            if lat:
                                P.op("dve", lambda: V.tensor_tensor(out=QK4[:].rearrange("p (r j) w -> p r j w", r=2), in0=qk.unsqueeze(1).to_broadcast([128, 2, 2, 128]),
                                                                    in1=fa[:, 0:4, :].rearrange("p (r j) w -> p r j w", r=2), op=ALU.mult),
                                     reads=[pqk, "FA%d" % pp], writes=["QK4"])
                            P.op("dve", lambda: V.tensor_tensor(out=KH[s][:, n, :, :], in0=kps.unsqueeze(1).to_broadcast([128, 2, 128]), in1=fa[:, 4:6, :], op=ALU.mult),
                                 reads=[pqk, "FA%d" % pp], writes=["KH%d_%d" % (s, n)])
                        steps.append(b0)
                    else:
                        rf = lambda i: cst[:, C_RF + h * 6 + i:C_RF + h * 6 + i + 1]
                        rfv = cst[:, C_RF + h * 6:C_RF + h * 6 + 6].rearrange("p (r c) -> p r c", r=2)
                        if lat:
                            csk = "CS%d" % pp
                            cosn = CS[pp][:, 0:128]
                            sinn = CS[pp][:, 128:256]
                            qk = pq[:, 0:256]

                            def rope():
                                P.op("dve", lambda: V.tensor_tensor(out=rt1[:], in0=qk.rearrange("p (j w) -> p j w", j=2), in1=cosn.unsqueeze(1).to_broadcast([128, 2, 128]), op=ALU.mult),
                                     reads=[pqk, csk], writes=["rt1"])
                                pv = qk.rearrange("p (j h s w) -> p (j h) s w", j=2, h=2, s=2, w=32)
                                sv = sinn.rearrange("p (h s w) -> p h s w", h=2, s=2, w=32)
                                ov = rt2[:].rearrange("p j (h s w) -> p (j h) s w", h=2, s=2, w=32)
                                for sidx in range(2):
                                    for j in range(2):
                                        P.op("dve", lambda sidx=sidx, j=j: V.tensor_tensor(
                                            out=ov[:, 2 * j:2 * j + 2, sidx, :], in0=pv[:, 2 * j:2 * j + 2, 1 - sidx, :], in1=sv[:, :, sidx, :], op=ALU.mult),
                                            reads=[pqk, csk], writes=["rt2"])
                                P.op("dve", lambda: V.tensor_tensor(out=qr[:], in0=rt1[:], in1=rt2[:], op=ALU.add), reads=["rt1", "rt2"], writes=["qr"])
                            steps.append(rope)
                            qkr = qr[:]
                            ka = qr[:, 1, :]
                            rk = ["qr"]
                        else:
                            qkr = None
                            ka = kps
                            rk = [pqk]

                        def b2r():
                            if lat:
                                P.op("dve", lambda: V.tensor_tensor(out=QK4[:].rearrange("p (r j) w -> p r j w", r=2), in0=qkr.unsqueeze(1).to_broadcast([128, 2, 2, 128]),
                                                                    in1=rfv[:, :, 0:2].unsqueeze(3).to_broadcast([128, 2, 2, 128]), op=ALU.mult),
                                     reads=rk + ["cst"], writes=["QK4"])
                            P.op("dve", lambda: V.tensor_tensor(out=KH[s][:, n, :, :], in0=ka.unsqueeze(1).to_broadcast([128, 2, 128]),
                                                                in1=rfv[:, :, 2:3].to_broadcast([128, 2, 128]), op=ALU.mult),
                                 reads=rk + ["cst"], writes=["KH%d_%d" % (s, n)])
                        steps.append(b2r)
                    if not lat:
                        return steps

                    def b1():
                        for i in range(4):
                            P.op("pe", lambda i=i: T.transpose(Bt[:, i * 128:(i + 1) * 128], QK4[:, i, :], ident_b), reads=["QK4", "cstb"], writes=["Bt"])

                    def b2():
                        btv = Bt[:, 0:512].rearrange("p (a b) -> p a b", a=4)
                        P.op("act", lambda: A.copy(out=QT[s][:, n, :, :], in_=btv[:, 0:3:2, :]), reads=["Bt"], writes=["QT%d_%d" % (s, n)])
                        P.op("dve", lambda: V.tensor_copy(out=KTt[:], in_=btv[:, 1:4:2, :]), reads=["Bt"], writes=["KTt"])

                    def b3():
                        for d in range(2):
                            P.op("pe", lambda d=d: T.matmul(B5[:, d * 128:(d + 1) * 128], KTt[:, d, :], QT[s][:, n, d, :], start=True, stop=True),
                                 reads=["KTt", "QT%d_%d" % (s, n)], writes=["B5"])

                    def b4():
                        P.op("dve", lambda: V.tensor_tensor(out=tAB[:], in0=B5[:, 0:256], in1=cst[:, C_LE:C_LE + 256], op=ALU.mult), reads=["B5", "cst"], writes=["tAB"])
                        P.op("dve", lambda: V.tensor_tensor(out=AT[s][:, n, :], in0=tAB[:, 0:128], in1=tAB[:, 128:256], op=ALU.add), reads=["tAB"], writes=["AT%d_%d" % (s, n)])
                    steps += [b1, b2, b3, b4]
                    return steps

                if not isg:
                    P.op("dve", lambda: V.tensor_copy(out=DFB[s][:], in_=cst[:, C_RD + 2 * h:C_RD + 2 * h + 2].unsqueeze(1).to_broadcast([128, NT + NCT, 2])),
                         reads=["cst"], writes=["DFB%d" % s])
                for st in stage1(0):
                    st()
                yield
                for n in range(NA):
                    sa = stage1(n + 1) if n + 1 < NA else []
                    sb_ = stage2(n)
                    for i in range(max(len(sa), len(sb_))):
                        if i < len(sa):
                            sa[i]()
                        if i < len(sb_):
                            sb_[i]()
                        yield

            sctr = [0]

            def state_step(s, n, d, stv, stkey, first):
                bi = 2 + (sctr[0] % 2)
                sctr[0] += 1
                bk = B[bi]
                bkey = "B%d" % bi
                P.op("pe", lambda: T.matmul(bk[:, 0:256], KH[s][:, n, d, :], VV[s][:, n, :], start=True, stop=True),
                     reads=["KH%d_%d" % (s, n), "VV%d_%d" % (s, n)], writes=[bkey])
                if first:
                    P.op("dve", lambda: V.tensor_copy(out=stv, in_=bk[:, 0:256]), reads=[bkey], writes=[stkey])
                else:
                    P.op("dve", lambda: V.scalar_tensor_tensor(out=stv, in0=stv, scalar=DFB[s][:, n, d:d + 1], in1=bk[:, 0:256], op0=ALU.mult, op1=ALU.add),
                         reads=[bkey, stkey, "DFB%d" % s], writes=[stkey])

            def passL(u):
                s = u % 2
                isg = u < 4
                h = u % 4
                state_step(s, NT, 0, SC[s][:, 0, :], "SC_0", True)
                state_step(s, NT + 1, 1, SC[s][:, 1, :], "SC_1", True)
                state_step(s, NT + 1, 0, SC[s][:, 0, :], "SC_0", False)
                state_step(s, NT, 1, SC[s][:, 1, :], "SC_1", False)
                for k in range(NT):
                    state_step(s, k, 0, XP[s][:, 0:256], "XP_0", k == 0)
                    state_step(s, NT - 1 - k, 1, XP[s][:, 256:512], "XP_1", k == 0)
                if isg:
                    P.op("dve", lambda: V.tensor_reduce(out=st8[:, 0:2], in_=TOT[s][:].rearrange("p n d -> p d n"), axis=AX.X, op=ALU.add),
                         reads=["TOT%d" % s], writes=["st8"])
                    P.op("act", lambda: A.activation(out=XP[s][:, 512:514], in_=st8[:, 0:2], func=AF.Exp, scale=-1.0 / TAU), reads=["st8"], writes=["XP_2"])
                else:
                    P.op("dve", lambda: V.tensor_copy(out=XP[s][:, 512:514], in_=cst[:, C_RDT + 2 * h:C_RDT + 2 * h + 2]), reads=["cst"], writes=["XP_2"])
                P.op("dve", lambda: V.memset(XP[s][:, 514:520], 0.0), writes=["XP_3"])
                P.dma("sp", cin_d[u].ap(), XP[s][:], reads=["XP_0", "XP_1", "XP_2", "XP_3"], writes=["cin%d" % u])
                P.collective("AllGather", ALU.bypass, [[0, 1, 2, 3], [4, 5, 6, 7]], [cin_d[u].ap().opt()], [cout_d[u].ap().opt()],
                             reads=["cin%d" % u], writes=["cout%d" % u])
                P.dma("pool", XG[s][:], cout_d[u].ap().rearrange("(r p) c -> p r c", p=128), reads=["cout%d" % u], writes=["XG"])

            def passBC(u):
                s = u % 2
                isg = u < 4
                h = u % 4
                for d in range(2):
                    P.op("dve", lambda d=d: V.tensor_copy(out=ST[:, d, :], in_=SC[s][:, d, :]), reads=["SC_%d" % d], writes=["ST%d" % d])
                    order = range(4) if d == 0 else range(3, -1, -1)
                    for i in order:
                        mcol = cst[:, C_CM + d * 4 + i:C_CM + d * 4 + i + 1]
                        P.op("dve", lambda d=d, i=i, mcol=mcol: V.tensor_scalar(out=dpr[:, 2 * d:2 * d + 1], in0=XG[s][:, i, 512 + d:513 + d], scalar1=-1.0, scalar2=mcol, op0=ALU.add, op1=ALU.mult),
                             reads=["XG", "cst"], writes=["dpr%d" % d])
                        P.op("dve", lambda d=d: V.tensor_scalar(out=dpr[:, 2 * d + 1:2 * d + 2], in0=dpr[:, 2 * d:2 * d + 1], scalar1=1.0, scalar2=None, op0=ALU.add), reads=["dpr%d" % d], writes=["dprb%d" % d])
                        P.op("dve", lambda d=d, i=i, mcol=mcol: V.tensor_scalar(out=xtmp[:], in0=XG[s][:, i, d * 256:(d + 1) * 256], scalar1=mcol, scalar2=None, op0=ALU.mult),
                             reads=["XG", "cst"], writes=["xtmp"])
                        P.op("dve", lambda d=d: V.scalar_tensor_tensor(out=ST[:, d, :], in0=ST[:, d, :], scalar=dpr[:, 2 * d + 1:2 * d + 2], in1=xtmp[:], op0=ALU.mult, op1=ALU.add),
                             reads=["dprb%d" % d, "xtmp", "ST%d" % d], writes=["ST%d" % d])
                        yield
                for n in range(NT - 1, -1, -1):
                    P.op("act", lambda n=n: A.copy(out=GS[:, n, :], in_=ST[:, 1, :]), reads=["ST1"], writes=["GS_%d" % n])
                    if n > 0:
                        state_step(s, n, 1, ST[:, 1, :], "ST1", False)
                    yield
                BO = [B6, B7]

                def mm(n):
                    pp = n % 2
                    bo = BO[pp]
                    bok = "B%d" % (6 + pp)
                    P.op("act", lambda: A.copy(out=STb[pp][:], in_=ST[:, 0, :]), reads=["ST0"], writes=["STb%d" % pp])
                    P.op("pe", lambda: T.matmul(bo[:, 0:256], AT[s][:, n, :], VV[s][:, n, :], start=True, stop=False),
                         reads=["AT%d_%d" % (s, n), "VV%d_%d" % (s, n)], writes=[bok])
                    P.op("pe", lambda: T.matmul(bo[:, 0:256], QT[s][:, n, 0, :], STb[pp][:], start=False, stop=False),
                         reads=["QT%d_%d" % (s, n), "STb%d" % pp], writes=[bok])
                    P.op("pe", lambda: T.matmul(bo[:, 0:256], QT[s][:, n, 1, :], GS[:, n, :], start=False, stop=True),
                         reads=["QT%d_%d" % (s, n), "GS_%d" % n], writes=[bok])
                    for kc in range(8):
                        P.op("pe", lambda kc=kc: T.matmul(bo[:, 256:512], hT[:, kc, n * 128:(n + 1) * 128], WGt[s][:, kc, :], start=(kc == 0), stop=(kc == 7)),
                             reads=["hT", "WG%d" % s], writes=[bok])
                    if n < NT - 1:
                        state_step(s, n, 0, ST[:, 0, :], "ST0", False)

                def epi(n):
                    pp = n % 2
                    bo = BO[pp]
                    bok = "B%d" % (6 + pp)
                    sg_, og_, ot_ = sgt[pp], ogt[pp], otmp[pp]
                    P.op("act", lambda: A.activation(out=sg_[:], in_=bo[:, 256:512], func=AF.Silu), reads=[bok], writes=["sgt%d" % pp])
                    if isg:
                        P.op("act", lambda: A.activation(out=ot_[:], in_=bo[:, 0:256], func=AF.Square, accum_out=st8[:, 4:5]), reads=[bok], writes=["otmp%d" % pp, "st8a"])
                        P.op("act", lambda: A.activation(out=st8[:, 5:6], in_=st8[:, 4:5], func=AF.Sqrt, bias=eps_ap, scale=1.0 / 256), reads=["st8a", "cst"], writes=["st8b"])
                        P.op("dve", lambda: V.reciprocal(out=st8[:, 6:7], in_=st8[:, 5:6]), reads=["st8b"], writes=["st8c"])
                        P.op("dve", lambda: V.scalar_tensor_tensor(out=og_[:], in0=bo[:, 0:256], scalar=st8[:, 6:7], in1=sg_[:], op0=ALU.mult, op1=ALU.mult),
                             reads=[bok, "st8c", "sgt%d" % pp], writes=["ogt%d" % pp])
                    else:
                        P.op("dve", lambda: V.bn_stats(out=st8[:, 8:14], in_=bo[:, 0:256]), reads=[bok], writes=["st8s"])
                        P.op("dve", lambda: V.bn_aggr(out=st8[:, 14:16], in_=st8[:, 8:14]), reads=["st8s"], writes=["st8m"])
                        P.op("act", lambda: A.activation(out=st8[:, 5:6], in_=st8[:, 15:16], func=AF.Sqrt, bias=eps_ap, scale=1.0), reads=["st8m", "cst"], writes=["st8b"])
                        P.op("dve", lambda: V.reciprocal(out=st8[:, 6:7], in_=st8[:, 5:6]), reads=["st8b"], writes=["st8c"])
                        P.op("dve", lambda: V.tensor_scalar(out=ot_[:], in0=bo[:, 0:256], scalar1=st8[:, 14:15], scalar2=st8[:, 6:7], op0=ALU.subtract, op1=ALU.mult),
                             reads=[bok, "st8m", "st8c"], writes=["otmp%d" % pp])
                        P.op("dve", lambda: V.tensor_tensor(out=og_[:], in0=ot_[:], in1=sg_[:], op=ALU.mult), reads=["otmp%d" % pp, "sgt%d" % pp], writes=["ogt%d" % pp])
                    for c in range(2):
                        P.op("pe", lambda c=c: T.transpose(Bt[:, 512 + c * 128:512 + (c + 1) * 128], og_[:, c * 128:(c + 1) * 128], ident_b), reads=["ogt%d" % pp, "cstb"], writes=["Bt"])
                    P.op("act", lambda: A.copy(out=OGT[s][:, :, n * 128:(n + 1) * 128], in_=Bt[:, 512:768].rearrange("p (c t) -> p c t", c=2)),
                         reads=["Bt"], writes=["OGT"])

                mm(0)
                yield
                for n in range(NT):
                    if n + 1 < NT:
                        mm(n + 1)
                        yield
                    epi(n)
                    yield
                P.dma("sp", ogT_d.ap()[u * 256:(u + 1) * 256, :].rearrange("(c p) t -> p c t", p=128), OGT[s][:], reads=["OGT"], writes=["ogT_d"])


            load_weights(0, "qkv")
            load_weights(0, "g")
            for u in range(8):
                if u + 1 < 8:
                    load_weights(u + 1, "qkv")
                ga = passA(u)
                gb = passBC(u - 1) if u >= 1 else iter(())
                da = db = False
                if u < 4:
                    for _ in ga:
                        pass
                    for _ in gb:
                        pass
                    da = db = True
                while not (da and db):
                    if not da:
                        try:
                            next(ga)
                        except StopIteration:
                            da = True
                    if not db:
                        try:
                            next(gb)
                        except StopIteration:
                            db = True
                if u == 0:
                    ck(2)
                if u + 1 < 8:
                    load_weights(u + 1, "g")
                passL(u)
                if u == 0:
                    ck(3)
                if u == 1:
                    ck(4)
            for _ in passBC(7):
                pass
            if "ogt_last" in dbg_out:
                pass
        P.barrier()
        ck(5)

        with contextlib.ExitStack() as s3:
            YH = sb(s3, "YH", [128, 8, SEG], BF16)
            r = lambda ap_: ap_.rearrange("(kc p) c -> p kc c", p=128)
            with contextlib.ExitStack() as s3a:
                nsc = {"sq": sb(s3a, "m_sq", [128, D], BF16)[:], "ss": sb(s3a, "m_ss", [128, 4])[:], "xs": sb(s3a, "m_xs", [128, D], BF16)[:]}
                WM = sb(s3a, "WM", [128, 8, 2048], BF16)
                WBG = sb(s3a, "WBG", [128, 8, D], BF16)
                WBR = sb(s3a, "WBR", [128, 8, D], BF16)
                OGB = [sb(s3a, "OGB%d" % i, [128, 16, 256], BF16) for i in range(2)]
                hTb = sb(s3a, "hTb", [128, 8, 256], BF16)
                xt3 = [sb(s3a, "x3_%d" % i, [128, D]) for i in range(2)]
                sg2 = [sb(s3a, "sg2_%d" % i, [128, 2, 256]) for i in range(2)]
                y12 = sb(s3a, "y12", [128, 2, 256])
                PM = [pbank(s3a, "PM%d" % i) for i in range(2)]
                PB = [pbank(s3a, "PB%d" % i) for i in range(2)]
                PT = pbank(s3a, "PT", BF16)
                P.dma("pool", WM[:], r(w_in_d[:, MG:MG + 2048]), writes=["WM"])
                P.dma("pool", WBG[:], r(wbg_d), writes=["WBG"])
                P.dma("pool", WBR[:], r(wbr_d), writes=["WBR"])
                for fc in range(8):
                    P.op("dve", lambda fc=fc: V.tensor_scalar(out=WBG[:, fc, :], in0=WBG[:, fc, :], scalar1=vec[:, VC_GN + fc % 2:VC_GN + fc % 2 + 1], scalar2=None, op0=ALU.mult),
                         reads=["WBG", "vec"], writes=["WBG"])
                for blk in range(8):
                    ob = OGB[blk % 2]
                    obk = "OGB%d" % (blk % 2)
                    P.dma("sp", ob[:], ogT_d.ap()[:, blk * 256:(blk + 1) * 256].rearrange("(c p) t -> p c t", p=128), writes=[obk])
                    for tt in range(2):
                        n = blk * 2 + tt
                        xt = xt3[n % 2]
                        xk = "x3_%d" % (n % 2)
                        P.dma("sp", xt[:], x_d[n * 128:(n + 1) * 128, :], writes=[xk])
                        norm_T(xt[:], xk, PT, "PT", lambda kc, tt=tt: hTb[:, kc, tt * 128:(tt + 1) * 128], "hTb", M_GM1, M_SH1, nsc)
                    for nch in range(8):
                        pm = PM[nch % 2]
                        pb = PB[nch % 2]
                        pmk = "PM%d" % (nch % 2)
                        pbk = "PB%d" % (nch % 2)
                        sg = sg2[nch % 2]
                        sgk = "sg2_%d" % (nch % 2)
                        for br_ in range(2):
                            for kc in range(8):
                                P.op("pe", lambda br_=br_, kc=kc, pm=pm, nch=nch: T.matmul(
                                    pm[:, br_ * 256:(br_ + 1) * 256], WM[:, kc, br_ * 1024 + nch * 128:br_ * 1024 + (nch + 1) * 128], hTb[:, kc, :],
                                    start=(kc == 0), stop=(kc == 7)), reads=["WM", "hTb"], writes=[pmk])
                        for br_ in range(2):
                            wb = WBG if br_ == 0 else WBR
                            for fc in range(8):
                                P.op("pe", lambda br_=br_, fc=fc, pb=pb, wb=wb, nch=nch, ob=ob: T.matmul(
                                    pb[:, br_ * 256:(br_ + 1) * 256], wb[:, fc, nch * 128:(nch + 1) * 128], ob[:, br_ * 8 + fc, :],
                                    start=(fc == 0), stop=(fc == 7)), reads=["WBG", "WBR", obk], writes=[pbk])
                        P.op("act", lambda pm=pm, sg=sg: A.activation(out=sg[:].rearrange("p a b -> p (a b)"), in_=pm[:, 0:512], func=AF.Sigmoid), reads=[pmk], writes=[sgk])
                        P.op("dve", lambda pb=pb, sg=sg: V.tensor_tensor(out=y12[:].rearrange("p a b -> p (a b)"), in0=pb[:, 0:512], in1=sg[:].rearrange("p a b -> p (a b)"), op=ALU.mult),
                             reads=[pbk, sgk], writes=["y12"])
                        P.op("dve", lambda nch=nch, blk=blk: V.tensor_tensor(out=YH[:, nch, blk * 256:(blk + 1) * 256], in0=y12[:, 0, :], in1=y12[:, 1, :], op=ALU.add),
                             reads=["y12"], writes=["YH_%d" % blk])
            P.barrier()
            ck(6)
            HL = sb(s3, "HL", [128, NT, D])
            with contextlib.ExitStack() as s3b:
                WO = sb(s3b, "WO", [128, 8, D], BF16)
                xt4 = [sb(s3b, "x4_%d" % i, [128, D]) for i in range(2)]
                otmp3 = sb(s3b, "otmp3", [128, D])
                PO = [pbank(s3b, "PO%d" % i) for i in range(4)]
                P.dma("pool", WO[:], r(wout_d), writes=["WO"])
                for n in range(NT):
                    xt = xt4[n % 2]
                    xk = "x4_%d" % (n % 2)
                    P.dma("sp", xt[:], x_d[n * 128:(n + 1) * 128, :], writes=[xk])
                    for half in range(2):
                        po = PO[(n % 2) * 2 + half]
                        pok = "PO%d" % ((n % 2) * 2 + half)
                        for kc in range(8):
                            P.op("pe", lambda kc=kc, po=po, half=half, n=n: T.matmul(
                                po[:, 0:512], YH[:, kc, n * 128:(n + 1) * 128], WO[:, kc, half * 512:(half + 1) * 512],
                                start=(kc == 0), stop=(kc == 7)), reads=["YH_%d" % (n // 2), "WO"], writes=[pok])
                        P.op("dve", lambda po=po, half=half: V.tensor_tensor(out=otmp3[:, half * 512:(half + 1) * 512], in0=po[:, 0:512], in1=g1bc[:, half * 512:(half + 1) * 512], op=ALU.mult),
                             reads=[pok, "g1bc"], writes=["otmp3_%d" % half])
                        P.op("dve", lambda n=n, half=half, xt=xt: V.tensor_tensor(out=HL[:, n, half * 512:(half + 1) * 512], in0=otmp3[:, half * 512:(half + 1) * 512], in1=xt[:, half * 512:(half + 1) * 512], op=ALU.add),
                             reads=["otmp3_%d" % half, xk], writes=["HL_%d" % n])
            P.barrier()
            ck(7)
            if "hl" in dbg_out:
                P.dma("sp", dbg_out["hl"].rearrange("(n p) d -> p n d", p=128), HL[:], reads=["HL_%d" % n for n in range(NT)], writes=["dbg_hl"])

            with contextlib.ExitStack() as s4:
                H2T = YH
                WR = sb(s4, "WR", [128, 8, 36])
                brow = sb(s4, "brow", [1, 36])
                WTs = sb(s4, "WTs", [128, NT, 32])
                h2f = sb(s4, "h2f", [128, 8, 128])
                xsf = sb(s4, "xsf", [128, D])
                sqf = sb(s4, "sqf", [128, D], BF16)
                L = sb(s4, "L", [128, 36])
                rs = sb(s4, "rs", [128, 16])
                r4 = sb(s4, "r4", [128, 8])
                elm = sb(s4, "elm", [128, 32])
                ex = sb(s4, "ex", [128, 32])
                sel = sb(s4, "sel", [128, 32])
                top8 = sb(s4, "top8", [128, 8])
                WGU = [sb(s4, "WGU%d" % i, [128, 2, 8, 512], BF16) for i in range(2)]
                WDn = [sb(s4, "WDn%d" % i, [128, 2, 2, D], BF16) for i in range(2)]
                sgm = [sb(s4, "sgm%d" % i, [128, 256]) for i in range(4)]
                hid = [sb(s4, "hid%d" % i, [128, 256], BF16) for i in range(4)]
                HIDT = [sb(s4, "HIDT%d" % i, [128, 2, 2, 128], BF16) for i in range(2)]
                yo = sb(s4, "yo", [128, D])
                PG = [pbank(s4, "PG%d" % i) for i in range(4)]
                PD = [pbank(s4, "PD%d" % i) for i in range(2)]
                PX = pbank(s4, "PX", BF16)
                PF = pbank(s4, "PF")
                P.dma("sp", WR[:], wr_d.rearrange("(kc p) c -> p kc c", p=128), writes=["WR"])
                P.dma("sp", brow[:], br_d, writes=["brow"])

                def load_experts(j):
                    bi = j % 2
                    for e in range(2):
                        ee = 2 * j + e
                        P.dma("pool", WGU[bi][:, e, :, 0:256], weg_d[ee].rearrange("(kc p) c -> p kc c", p=128), writes=["WGU%d" % bi])
                        P.dma("pool", WGU[bi][:, e, :, 256:512], weu_d[ee].rearrange("(kc p) c -> p kc c", p=128), writes=["WGU%d" % bi])
                        P.dma("pool", WDn[bi][:, e, :, :], wed_d[ee].rearrange("(fc p) c -> p fc c", p=128), writes=["WDn%d" % bi])

                load_experts(0)
                for n in range(NT):
                    P.op("act", lambda n=n: A.activation(out=sqf[:], in_=HL[:, n, :], func=AF.Square, accum_out=rs[:, 0:1]), reads=["HL_%d" % n], writes=["sqf", "rs0"])
                    P.op("act", lambda: A.activation(out=rs[:, 1:2], in_=rs[:, 0:1], func=AF.Sqrt, bias=eps_ap, scale=1.0 / D), reads=["rs0", "cst"], writes=["rs1"])
                    P.op("dve", lambda: V.reciprocal(out=rs[:, 2:3], in_=rs[:, 1:2]), reads=["rs1"], writes=["rs2"])
                    P.op("dve", lambda n=n: V.tensor_scalar(out=xsf[:], in0=HL[:, n, :], scalar1=rs[:, 2:3], scalar2=None, op0=ALU.mult), reads=["HL_%d" % n, "rs2"], writes=["xsf"])
                    for kc in range(8):
                        pd = PD[kc // 4]
                        P.op("pe", lambda kc=kc, pd=pd: T.transpose(pd[:, (kc % 4) * 128:(kc % 4 + 1) * 128], xsf[:, kc * 128:(kc + 1) * 128], ident_f),
                             reads=["xsf", "cst"], writes=["PD%d" % (kc // 4)])
                    for kc in range(8):
                        pd = PD[kc // 4]
                        src = pd[:, (kc % 4) * 128:(kc % 4 + 1) * 128]
                        if kc % 2 == 0:
                            P.op("act", lambda kc=kc, src=src: A.activation(out=h2f[:, kc, :], in_=src, func=AF.Identity, scale=mod[:, M_GM2 + kc:M_GM2 + kc + 1], bias=mod[:, M_SH2 + kc:M_SH2 + kc + 1]),
                                 reads=["PD%d" % (kc // 4), "mod"], writes=["h2f"])
                        else:
                            P.op("dve", lambda kc=kc, src=src: V.tensor_scalar(out=h2f[:, kc, :], in0=src, scalar1=mod[:, M_GM2 + kc:M_GM2 + kc + 1], scalar2=mod[:, M_SH2 + kc:M_SH2 + kc + 1], op0=ALU.mult, op1=ALU.add),
                                 reads=["PD%d" % (kc // 4), "mod"], writes=["h2f"])
                    P.op("dve", lambda n=n: V.tensor_copy(out=H2T[:, :, n * 128:(n + 1) * 128], in_=h2f[:]), reads=["h2f"], writes=["H2T_%d" % n])
                    for kc in range(8):
                        P.op("pe", lambda kc=kc: T.matmul(PF[:, 0:36], h2f[:, kc, :], WR[:, kc, :], start=(kc == 0), stop=False), reads=["h2f", "WR"], writes=["PF"])
                    P.op("pe", lambda: T.matmul(PF[:, 0:36], cst[0:1, C_ONE:C_ONE + 128], brow[0:1, :], start=False, stop=True), reads=["cst", "brow"], writes=["PF"])
                    P.op("dve", lambda: V.tensor_copy(out=L[:], in_=PF[:, 0:36]), reads=["PF"], writes=["L"])
                    P.op("dve", lambda: V.tensor_reduce(out=rs[:, 4:5], in_=L[:, 0:4], axis=AX.X, op=ALU.max), reads=["L"], writes=["rs4"])
                    P.op("dve", lambda: V.tensor_scalar(out=r4[:, 0:4], in0=L[:, 0:4], scalar1=rs[:, 4:5], scalar2=None, op0=ALU.is_equal), reads=["L", "rs4"], writes=["r4a"])
                    P.op("dve", lambda: V.tensor_scalar(out=rs[:, 5:6], in0=rs[:, 4:5], scalar1=-1.0, scalar2=None, op0=ALU.mult), reads=["rs4"], writes=["rs5"])
                    P.op("act", lambda: A.activation(out=r4[:, 4:8], in_=L[:, 0:4], func=AF.Exp, bias=rs[:, 5:6], accum_out=rs[:, 6:7]), reads=["L", "rs5"], writes=["r4b", "rs6"])
                    P.op("dve", lambda: V.reciprocal(out=rs[:, 7:8], in_=rs[:, 6:7]), reads=["rs6"], writes=["rs7"])
                    P.op("dve", lambda: V.tensor_scalar(out=r4[:, 0:4], in0=r4[:, 0:4], scalar1=BIG, scalar2=-BIG, op0=ALU.mult, op1=ALU.add), reads=["r4a"], writes=["r4a"])
                    P.op("dve", lambda: V.tensor_tensor(out=elm[:].rearrange("p (g e) -> p g e", g=4), in0=L[:, 4:36].rearrange("p (g e) -> p g e", g=4),
                                                        in1=r4[:, 0:4].unsqueeze(2).to_broadcast([128, 4, 8]), op=ALU.add), reads=["L", "r4a"], writes=["elm"])
                    P.op("dve", lambda: V.max(out=top8[:], in_=elm[:]), reads=["elm"], writes=["top8"])
                    P.op("dve", lambda: V.tensor_scalar(out=sel[:], in0=elm[:], scalar1=top8[:, 1:2], scalar2=None, op0=ALU.is_ge), reads=["elm", "top8"], writes=["sel"])
                    P.op("dve", lambda: V.tensor_scalar(out=rs[:, 8:9], in0=top8[:, 0:1], scalar1=-1.0, scalar2=None, op0=ALU.mult), reads=["top8"], writes=["rs8"])
                    P.op("act", lambda: A.activation(out=ex[:], in_=elm[:], func=AF.Exp, bias=rs[:, 8:9]), reads=["elm", "rs8"], writes=["ex"])
                    P.op("act", lambda: A.activation(out=rs[:, 9:10], in_=top8[:, 1:2], func=AF.Exp, bias=rs[:, 8:9]), reads=["top8", "rs8"], writes=["rs9"])
                    P.op("dve", lambda: V.tensor_scalar(out=rs[:, 10:11], in0=rs[:, 9:10], scalar1=1.0, scalar2=None, op0=ALU.add), reads=["rs9"], writes=["rs10"])
                    P.op("dve", lambda: V.reciprocal(out=rs[:, 11:12], in_=rs[:, 10:11]), reads=["rs10"], writes=["rs11"])
                    P.op("dve", lambda: V.tensor_tensor(out=rs[:, 12:13], in0=rs[:, 11:12], in1=rs[:, 7:8], op=ALU.mult), reads=["rs11", "rs7"], writes=["rs12"])
                    P.op("dve", lambda n=n: V.scalar_tensor_tensor(out=WTs[:, n, :], in0=ex[:], scalar=rs[:, 12:13], in1=sel[:], op0=ALU.mult, op1=ALU.mult),
                         reads=["ex", "sel", "rs12"], writes=["WTs_%d" % n])
                if "wts" in dbg_out:
                    P.dma("sp", dbg_out["wts"].rearrange("(n p) e -> p n e", p=128), WTs[:], reads=["WTs_%d" % n for n in range(NT)], writes=["dbg_wts"])
                for j in range(16):
                    bi = j % 2
                    if j + 1 < 16:
                        load_experts(j + 1)
                    for e in range(2):
                        for fc in range(2):
                            P.op("dve", lambda e=e, fc=fc, bi=bi: V.tensor_tensor(out=WDn[bi][:, e, fc, :], in0=WDn[bi][:, e, fc, :], in1=g2bc[:], op=ALU.mult),
                                 reads=["WDn%d" % bi, "g2bc"], writes=["WDn%d" % bi])
                    def gu(n):
                        np_ = n % 2
                        for e in range(2):
                            pg = PG[np_ * 2 + e]
                            pgk = "PG%d" % (np_ * 2 + e)
                            for kc in range(8):
                                P.op("pe", lambda e=e, kc=kc, pg=pg: T.matmul(pg[:, 0:512], H2T[:, kc, n * 128:(n + 1) * 128], WGU[bi][:, e, kc, :], start=(kc == 0), stop=(kc == 7)),
                                     reads=["H2T_%d" % n, "WGU%d" % bi], writes=[pgk])
                        for e in range(2):
                            pg = PG[np_ * 2 + e]
                            pgk = "PG%d" % (np_ * 2 + e)
                            sg_ = sgm[np_ * 2 + e]
                            hd_ = hid[np_ * 2 + e]
                            P.op("act", lambda pg=pg, sg_=sg_: A.activation(out=sg_[:], in_=pg[:, 0:256], func=AF.Silu), reads=[pgk], writes=["sgm%d" % (np_ * 2 + e)])
                            P.op("dve", lambda e=e, pg=pg, sg_=sg_, hd_=hd_: V.scalar_tensor_tensor(out=hd_[:], in0=sg_[:], scalar=WTs[:, n, 2 * j + e:2 * j + e + 1], in1=pg[:, 256:512], op0=ALU.mult, op1=ALU.mult),
                                 reads=["sgm%d" % (np_ * 2 + e), pgk, "WTs_%d" % n], writes=["hid%d" % (np_ * 2 + e)])

                    def rest(n):
                        np_ = n % 2
                        hb = np_
                        for e in range(2):
                            hd_ = hid[np_ * 2 + e]
                            for fc in range(2):
                                P.op("pe", lambda e=e, fc=fc, hd_=hd_: T.transpose(PX[:, (e * 2 + fc) * 128:(e * 2 + fc + 1) * 128], hd_[:, fc * 128:(fc + 1) * 128], ident_b),
                                     reads=["hid%d" % (np_ * 2 + e), "cstb"], writes=["PX"])
                        P.op("act", lambda: A.copy(out=HIDT[hb][:].rearrange("p e c t -> p (e c) t"), in_=PX[:, 0:512].rearrange("p (c t) -> p c t", c=4)), reads=["PX"], writes=["HIDT%d" % hb])
                        for half in range(2):
                            pd = PD[half]
                            pdk = "PD%d" % half
                            i = 0
                            for e in range(2):
                                for fc in range(2):
                                    P.op("pe", lambda e=e, fc=fc, pd=pd, half=half, i=i: T.matmul(pd[:, 0:512], HIDT[hb][:, e, fc, :], WDn[bi][:, e, fc, half * 512:(half + 1) * 512], start=(i == 0), stop=(i == 3)),
                                         reads=["HIDT%d" % hb, "WDn%d" % bi], writes=[pdk])
                                    i += 1
                            P.op("dve", lambda half=half, pd=pd: V.tensor_tensor(out=HL[:, n, half * 512:(half + 1) * 512], in0=HL[:, n, half * 512:(half + 1) * 512], in1=pd[:, 0:512], op=ALU.add),
                                 reads=[pdk, "HL_%d" % n], writes=["HL_%d" % n])

                    gu(0)
                    for n in range(NT):
                        if n + 1 < NT:
                            gu(n + 1)
                        rest(n)
                for n in range(NT):
                    P.op("act", lambda n=n: A.activation(out=sqf[:], in_=HL[:, n, :], func=AF.Square, accum_out=rs[:, 0:1]), reads=["HL_%d" % n], writes=["sqf", "rs0"])
                    P.op("act", lambda: A.activation(out=rs[:, 1:2], in_=rs[:, 0:1], func=AF.Sqrt, bias=eps_ap, scale=1.0 / D), reads=["rs0", "cst"], writes=["rs1"])
                    P.op("dve", lambda: V.reciprocal(out=rs[:, 2:3], in_=rs[:, 1:2]), reads=["rs1"], writes=["rs2"])
                    P.op("dve", lambda n=n: V.scalar_tensor_tensor(out=yo[:], in0=HL[:, n, :], scalar=rs[:, 2:3], in1=nfbc[:], op0=ALU.mult, op1=ALU.mult),
                         reads=["HL_%d" % n, "rs2", "nfbc"], writes=["yo"])
                    P.dma("sp", y_d[n * 128:(n + 1) * 128, :], yo[:], reads=["yo"], writes=["y_%d" % n])
                P.wait_all("sp")
                P.wait_all("act")
    return nc, P


_CACHE = {}


def _get_program():
    if "nc" not in _CACHE:
        _, P1 = build_program(None)
        nc, P2 = build_program(P1.needed_out)
        _CACHE["nc"] = nc
    return _CACHE["nc"]


def _host_consts(j):
    c = np.zeros((128, NCST), np.float64)
    idx = np.arange(128)
    s_, t_ = idx[:, None], idx[None, :]
    c[:, C_ID:C_ID + 128] = np.eye(128)
    c[:, C_LE:C_LE + 128] = (s_ <= t_)
    c[:, C_GT:C_GT + 128] = (s_ > t_)
    c[:, C_GE:C_GE + 128] = (s_ >= t_)
    c[:, C_LT:C_LT + 128] = (s_ < t_)
    c[:, C_ONE:C_ONE + 128] = 1.0
    ks = 128.0 ** -0.5
    t = idx.astype(np.float64)
    for h in range(4):
        lf = np.log1p(-2.0 ** (-5.0 - h))
        lb = np.log1p(-2.0 ** (-5.5 - h))
        f = [np.exp(lf * (t + 1)), np.exp(-lf * (t + 1)) * ks, np.exp(lf * (127 - t)) * ks,
             np.exp(lb * (128 - t)), np.exp(-lb * (128 - t)) * ks, np.exp(lb * t) * ks]
        for i in range(6):
            c[:, C_RF + h * 6 + i] = f[i]
        c[:, C_RD + 2 * h] = np.exp(lf * 128)
        c[:, C_RD + 2 * h + 1] = np.exp(lb * 128)
        c[:, C_RDT + 2 * h] = np.exp(lf * SEG)
        c[:, C_RDT + 2 * h + 1] = np.exp(lb * SEG)
    for i in range(4):
        c[:, C_CM + i] = 1.0 if i < j else 0.0
        c[:, C_CM + 4 + i] = 1.0 if i > j else 0.0
    c[:, C_EPS] = EPS
    c[:, C_EPS + 1] = np.log(128.0 ** -0.5)
    inv = (10000.0 ** (-np.arange(32, dtype=np.float32) / np.float32(32))).astype(np.float32)
    for n in range(NT):
        pos = SEG * j + n * 128 + idx
        row = (pos // 64).astype(np.float32)
        col = (pos % 64).astype(np.float32)
        ar = (row[:, None] * inv[None, :]).astype(np.float32).astype(np.float64)
        ac = (col[:, None] * inv[None, :]).astype(np.float32).astype(np.float64)
        c[:, C_CS + n * 256:C_CS + n * 256 + 128] = np.concatenate([np.cos(ar), np.cos(ar), np.cos(ac), np.cos(ac)], axis=1)
        c[:, C_CS + n * 256 + 128:C_CS + (n + 1) * 256] = np.concatenate([-np.sin(ar), np.sin(ar), -np.sin(ac), np.sin(ac)], axis=1)
    return c.astype(np.float32)


def make_in_maps(x, c, ctx, c_ctx, w_ada, b_ada, norm_mix, norm_ffn, w_in, gla_lr_w, gla_lr_b, gla_norm,
                 w_branch_gla, w_branch_ret, w_out, w_router_group, b_router_group, w_router_expert,
                 b_router_expert, w_expert_gate, w_expert_up, w_expert_down, norm_final):
    f = lambda a: np.ascontiguousarray(np.asarray(a, dtype=np.float32))
    x, c, ctx, c_ctx = f(x), f(c), f(ctx), f(c_ctx)
    fm = lambda v: np.ascontiguousarray(v.reshape(-1, 128).T)
    b_ada0 = f(b_ada)[0]
    shared = {
        "w_ada": f(w_ada)[0], "w_in": f(w_in)[0],
        "lrw": np.ascontiguousarray(np.concatenate([f(gla_lr_w)[0].transpose(1, 0, 2), f(gla_lr_b)[0][None]], axis=0)),
        "w_branch_gla": f(w_branch_gla)[0], "w_branch_ret": f(w_branch_ret)[0], "w_out": f(w_out)[0],
        "w_router": np.ascontiguousarray(np.concatenate([f(w_router_group)[0], f(w_router_expert)[0]], axis=1)),
        "b_router": np.ascontiguousarray(np.concatenate([f(b_router_group)[0], f(b_router_expert)[0]])[None, :]),
        "w_expert_gate": f(w_expert_gate)[0], "w_expert_up": f(w_expert_up)[0], "w_expert_down": f(w_expert_down)[0],
    }
    gn_row = np.zeros((D,), np.float32)
    gn_row[:256] = f(gla_norm)[0]
    maps = []
    for i in range(8):
        b, j = i // 4, i % 4
        vec = np.concatenate([fm(c[b]), fm(c_ctx), fm(b_ada0[0:1024]), fm(b_ada0[1024:2048]), fm(b_ada0[3072:4096]),
                              fm(b_ada0[4096:5120]), fm(f(norm_mix)[0]), fm(f(norm_ffn)[0]), fm(f(gla_norm)[0]),
                              np.zeros((128, 6), np.float32)], axis=1)
        rowv = np.stack([b_ada0[2048:3072], b_ada0[5120:6144], f(norm_final), gn_row], axis=0)
        m = dict(shared)
        m.update({"x": np.ascontiguousarray(x[b, j * SEG:(j + 1) * SEG]), "ctx": np.ascontiguousarray(ctx[b]),
                  "cst": _host_consts(j), "vec": np.ascontiguousarray(vec), "rowv": np.ascontiguousarray(rowv)})
        maps.append(m)
    return maps


def kernel(**inputs):
    nc = _get_program()
    maps = make_in_maps(**inputs)
    res = run_bass_kernel_spmd(nc, maps, core_ids=list(range(8)))
    out = np.empty((2, 4 * SEG, D), np.float32)
    for i in range(8):
        out[i // 4, (i % 4) * SEG:(i % 4 + 1) * SEG] = res.results[i]["y"]
    return out
```

```python
import contextlib
import numpy as np
import concourse.bass as bass
import concourse.mybir as mybir
from concourse.bass_utils import run_bass_kernel_spmd

F32 = mybir.dt.float32
BF16 = mybir.dt.bfloat16
AF = mybir.ActivationFunctionType
ALU = mybir.AluOpType
AX = mybir.AxisListType

D = 1024
SEG = 2048
NT = 16
CTX = 256
NCT = 2
GQ, GK, GV, GG, LRF, LRB, RQ, RK, RV, RG, MG, MR = 0, 512, 1024, 2048, 3072, 3088, 3104, 3616, 4128, 5152, 6176, 7200
EPS = 1e-6
TAU = 16.0
BIG = 30000.0

C_ID, C_LE, C_GT, C_GE, C_LT, C_ONE = 0, 128, 256, 384, 512, 640
C_RF = 768
C_RD = 792
C_RDT = 800
C_CM = 808
C_EPS = 816
C_CS = 832
NCS = 832
NCST = C_CS + NT * 256
NCB = 768

COMPUTE = ("pe", "act", "dve", "pool")
import re as _re
_PSUM_RE = _re.compile(r"^(B\d|Bt|pa|pg\d|PM\d|PB\d|PO\d|PT|PG\d|PD\d|PX|PF)$")


class Prog:
    NDMA = 32

    def __init__(self, nc, es, needed=None):
        self.nc = nc
        self.learn = needed is None
        self.needed_in = needed or set()
        self.needed_out = set()
        self.eng = {"pe": nc.tensor, "act": nc.scalar, "dve": nc.vector, "pool": nc.gpsimd, "sp": nc.sync}
        self.sem = {e: es.enter_context(nc.semaphore("s_" + e)) for e in COMPUTE}
        self.dsem = [es.enter_context(nc.semaphore("s_dma%d" % i)) for i in range(self.NDMA)]
        self.qn = {"sp": 0, "pool": 0, "act": 0}
        self.csem = es.enter_context(nc.semaphore("s_cc"))
        self.dcnt = [0] * self.NDMA
        self.dlast = [None] * self.NDMA
        self.ccnt = 0
        self.nd = 0
        self.seq = {e: 0 for e in COMPUTE}
        self.ops = []
        self.lastw = {}
        self.readers = {}
        self.waited = {e: {} for e in self.eng}
        self.last_on = {e: None for e in self.eng}
        self.nwait = 0
        self.kn = {e: {} for e in self.eng}
        self.clk = []

    def _merge(self, eng, opid):
        k = self.kn[eng]
        for e, v in self.clk[opid].items():
            if k.get(e, -1) < v:
                k[e] = v

    def _wait(self, eng, opid):
        kind, sem, val, oeng = self.ops[opid]
        if kind == "c" and self.kn[eng].get(oeng, -1) >= opid:
            return
        if val is None:
            return
        w = self.waited[eng]
        key = id(sem)
        if w.get(key, 0) >= val:
            self._merge(eng, opid)
            return
        w[key] = val
        self.needed_out.add(opid)
        self.eng[eng].wait_ge(sem, val)
        self.nwait += 1
        self._merge(eng, opid)

    def _deps(self, reads, writes):
        deps = set()
        for b in reads:
            if b in self.lastw:
                deps.add(self.lastw[b])
        for b in writes:
            if b in self.lastw:
                deps.add(self.lastw[b])
            for r in self.readers.get(b, ()):
                deps.add(r)
        return deps

    def _commit(self, opid, reads, writes):
        for b in reads:
            self.readers.setdefault(b, []).append(opid)
        for b in writes:
            self.lastw[b] = opid
            self.readers[b] = []

    def op(self, eng, fn, reads=(), writes=()):
        pr = [b for b in reads if _PSUM_RE.match(b)]
        if pr:
            writes = list(writes) + [b for b in pr if b not in writes]
            reads = [b for b in reads if b not in pr]
        opid = len(self.ops)
        raw = {self.lastw[b] for b in reads if b in self.lastw}
        for d in sorted(self._deps(reads, writes)):
            k, s, v, oe = self.ops[d]
            if oe == eng and k == "c" and (eng == "pe" or d not in raw):
                continue
            self._wait(eng, d)
        ins = fn()
        if self.learn or (opid in self.needed_in):
            self.seq[eng] += 1
            ins.then_inc(self.sem[eng], 1)
            self.ops.append(("c", self.sem[eng], self.seq[eng], eng))
        else:
            self.ops.append(("c", self.sem[eng], None, eng))
        c = dict(self.kn[eng])
        c[eng] = opid
        self.clk.append(c)
        self._commit(opid, reads, writes)
        self.last_on[eng] = opid
        return ins

    def dma(self, q, out, in_, reads=(), writes=(), **kw):
        opid = len(self.ops)
        for d in sorted(self._deps(reads, writes)):
            self._wait(q, d)
        half = self.NDMA // 2
        if q == "pool":
            i = half + self.qn[q] % half
        else:
            i = self.qn[q] % half
        self.qn[q] += 1
        self.nd += 1
        if self.dlast[i] is not None:
            self._wait(q, self.dlast[i])
        ins = self.eng[q].dma_start(out=out, in_=in_, **kw)
        self.dcnt[i] += 16
        ins.then_inc(self.dsem[i], 16)
        self.ops.append(("d", self.dsem[i], self.dcnt[i], q))
        self.clk.append(dict(self.kn[q]))
        self.dlast[i] = opid
        self._commit(opid, reads, writes)
        return ins

    def collective(self, kind, alu, groups, ins_, outs_, reads=(), writes=()):
        opid = len(self.ops)
        for d in sorted(self._deps(reads, writes)):
            self._wait("pool", d)
        ins = self.nc.gpsimd.collective_compute(kind, alu, replica_groups=groups, ins=ins_, outs=outs_)
        self.ccnt += 1
        ins.then_inc(self.csem, 1)
        self.ops.append(("x", self.csem, self.ccnt, "pool"))
        self.clk.append(dict(self.kn["pool"]))
        self._commit(opid, reads, writes)
        return ins

    def barrier(self):
        lasts = [self.last_on[e] for e in COMPUTE if self.last_on[e] is not None]
        lasts += [d for d in self.dlast if d is not None]
        for e in self.eng:
            for d in lasts:
                k, s, v, oe = self.ops[d]
                if oe == e and k == "c":
                    continue
                self._wait(e, d)
        self.lastw.clear()
        self.readers.clear()

    def wait_all(self, eng):
        for d in self.dlast:
            if d is not None:
                self._wait(eng, d)
        for e in COMPUTE:
            if self.last_on[e] is not None and e != eng:
                self._wait(eng, self.last_on[e])


class _Stop(Exception):
    pass


def build_program(needed=None, dbg=None, stop=None):
    try:
        return _build(needed, dbg, stop)
    except _Stop as e:
        return e.args


def _build(needed=None, dbg=None, stop=None):
    nc = bass.Bass("TRN2", target_bir_lowering=False)

    def din(name, shape, dt=F32):
        return nc.dram_tensor(name, list(shape), dt, kind="ExternalInput").ap()

    x_d = din("x", [SEG, D])
    ctx_d = din("ctx", [CTX, D])
    cst_d = din("cst", [128, NCST])
    vec_d = din("vec", [128, 72])
    rowv_d = din("rowv", [4, D])
    w_ada_d = din("w_ada", [D, 6 * D])
    w_in_d = din("w_in", [D, 8224])
    lrw_d = din("lrw", [17, 2, 512])
    wbg_d = din("w_branch_gla", [D, D])
    wbr_d = din("w_branch_ret", [D, D])
    wout_d = din("w_out", [D, D])
    wr_d = din("w_router", [D, 36])
    br_d = din("b_router", [1, 36])
    if stop is None or stop == 7:
        weg_d = din("w_expert_gate", [32, D, 256])
        weu_d = din("w_expert_up", [32, D, 256])
        wed_d = din("w_expert_down", [32, 256, D])
    y_d = nc.dram_tensor("y", [SEG, D], F32, kind="ExternalOutput").ap()
    dbg_out = {}
    if dbg:
        for k, shp in dbg.items():
            dbg_out[k] = nc.dram_tensor("dbg_" + k, list(shp), F32, kind="ExternalOutput").ap()

    ogT_d = nc.dram_tensor("ogT_scratch", [16 * 128, SEG], BF16)
    cin_d = [nc.dram_tensor("cc_in%d" % u, [128, 520], F32) for u in range(8)]
    cout_d = [nc.dram_tensor("cc_out%d" % u, [4 * 128, 520], F32) for u in range(8)]

    with contextlib.ExitStack() as es:
        P = Prog(nc, es, needed)
        V = nc.vector
        A = nc.scalar
        T = nc.tensor

        def sb(st, name, shape, dt=F32):
            return st.enter_context(nc.sbuf_tensor("sb_" + name, list(shape), dt))

        def ck(k):
            if stop == k:
                P.wait_all("sp")
                raise _Stop(nc, P)

        def pbank(st, name, dt=F32):
            return st.enter_context(nc.psum_tensor("ps_" + name, [128, 512 if dt == F32 else 1024], dt))

        cst = sb(es, "cst", [128, NCS])
        cstb = sb(es, "cstb", [128, NCB], BF16)
        vec = sb(es, "vec", [128, 72])
        mod = sb(es, "mod", [128, 48])
        g1bc = sb(es, "g1bc", [128, D])
        g2bc = sb(es, "g2bc", [128, D])
        nfbc = sb(es, "nfbc", [128, D])
        P.dma("sp", cst[:], cst_d[:, 0:NCS], writes=["cst"])
        P.dma("pool", cstb[:], cst_d[:, 0:NCB], writes=["cstb"])
        P.dma("sp", vec[:], vec_d, writes=["vec"])
        P.dma("sp", g1bc[:], rowv_d[0:1, :].partition_broadcast(128), writes=["g1bc"])
        P.dma("sp", g2bc[:], rowv_d[1:2, :].partition_broadcast(128), writes=["g2bc"])
        P.dma("sp", nfbc[:], rowv_d[2:3, :].partition_broadcast(128), writes=["nfbc"])
        ident_f = cst[:, C_ID:C_ID + 128]
        ident_b = cstb[:, C_ID:C_ID + 128]
        eps_ap = cst[:, C_EPS:C_EPS + 1]
        lnqs_ap = cst[:, C_EPS + 1:C_EPS + 2]
        VC_C, VC_CC, VC_BSH1, VC_BSC1, VC_BSH2, VC_BSC2, VC_NM, VC_NF, VC_GN = 0, 8, 16, 24, 32, 40, 48, 56, 64
        M_GM1, M_SH1, M_CGM1, M_CSH1, M_GM2, M_SH2 = 0, 8, 16, 24, 32, 40

        with contextlib.ExitStack() as s0:
            wa = [sb(s0, "wa%d" % i, [128, 8, 512]) for i in range(2)]
            sil = sb(s0, "sil", [128, 16])
            silbc = sb(s0, "silbc", [128, 8, 128])
            pa = pbank(s0, "pa")
            pg = [pbank(s0, "pg%d" % i) for i in range(2)]
            P.op("act", lambda: A.activation(out=sil[:], in_=vec[:, 0:16], func=AF.Silu), reads=["vec"], writes=["sil"])
            P.op("dve", lambda: V.tensor_copy(out=silbc[:], in_=sil[:, 0:8].unsqueeze(2).to_broadcast([128, 8, 128])),
                 reads=["sil"], writes=["silbc"])
            gi = 0
            for grp in (0, 1, 3, 4, 2, 5):
                for half in range(2):
                    buf = wa[gi % 2]
                    key = "wa%d" % (gi % 2)
                    gi += 1
                    c0 = grp * 1024 + half * 512
                    P.dma("sp", buf[:], w_ada_d[:, c0:c0 + 512].rearrange("(kc p) c -> p kc c", p=128), writes=[key])
                    if grp in (0, 1, 3, 4):
                        for cc in range(4):
                            ch = half * 4 + cc
                            col = grp * 16 + ch * 2
                            for kc in range(8):
                                P.op("pe", lambda cc=cc, kc=kc, col=col, buf=buf: T.matmul(
                                    pa[:, col:col + 2], buf[:, kc, cc * 128:(cc + 1) * 128],
                                    sil[:, kc:kc + 9:8], start=(kc == 0), stop=(kc == 7)),
                                    reads=[key, "sil"], writes=["pa"])
                    else:
                        pgt = pg[half]
                        for kc in range(8):
                            P.op("pe", lambda kc=kc, buf=buf, pgt=pgt: T.matmul(
                                pgt[:, 0:512], silbc[:, kc, :], buf[:, kc, :], start=(kc == 0), stop=(kc == 7)),
                                reads=[key, "silbc"], writes=["pg%d" % half])
                        dst = g1bc if grp == 2 else g2bc
                        dk = "g1bc" if grp == 2 else "g2bc"
                        P.op("dve", lambda dst=dst, pgt=pgt, half=half: V.tensor_tensor(
                            out=dst[:, half * 512:(half + 1) * 512], in0=pgt[:, 0:512],
                            in1=dst[:, half * 512:(half + 1) * 512], op=ALU.add),
                            reads=["pg%d" % half, dk], writes=[dk])
            pav = pa[:, 0:96].rearrange("p (g c w) -> p g c w", g=6, c=8, w=2)
            tmpm = sb(s0, "tmpm", [128, 48])
            P.op("dve", lambda: V.tensor_tensor(out=mod[:, M_SH1:M_SH1 + 8], in0=pav[:, 0, :, 0], in1=vec[:, VC_BSH1:VC_BSH1 + 8], op=ALU.add), reads=["pa", "vec"], writes=["mod"])
            P.op("dve", lambda: V.tensor_tensor(out=mod[:, M_CSH1:M_CSH1 + 8], in0=pav[:, 0, :, 1], in1=vec[:, VC_BSH1:VC_BSH1 + 8], op=ALU.add), reads=["pa", "vec"], writes=["mod"])
            P.op("dve", lambda: V.tensor_tensor(out=mod[:, M_SH2:M_SH2 + 8], in0=pav[:, 3, :, 0], in1=vec[:, VC_BSH2:VC_BSH2 + 8], op=ALU.add), reads=["pa", "vec"], writes=["mod"])
            for (dstc, grp, w, bcol, ncol) in ((M_GM1, 1, 0, VC_BSC1, VC_NM), (M_CGM1, 1, 1, VC_BSC1, VC_NM), (M_GM2, 4, 0, VC_BSC2, VC_NF)):
                P.op("dve", lambda grp=grp, w=w, bcol=bcol: V.scalar_tensor_tensor(
                    out=tmpm[:, 0:8], in0=pav[:, grp, :, w], scalar=1.0, in1=vec[:, bcol:bcol + 8], op0=ALU.add, op1=ALU.add),
                    reads=["pa", "vec"], writes=["tmpm"])
                P.op("dve", lambda dstc=dstc, ncol=ncol: V.tensor_tensor(
                    out=mod[:, dstc:dstc + 8], in0=tmpm[:, 0:8], in1=vec[:, ncol:ncol + 8], op=ALU.mult),
                    reads=["tmpm", "vec"], writes=["mod"])
        P.barrier()
        ck(0)

        def norm_T(xt, xkey, ptr, ptrkey, dst_fn, dstkey, gmc, shc, sc):
            sq, ss, xs = sc["sq"], sc["ss"], sc["xs"]
            P.op("act", lambda: A.activation(out=sq, in_=xt, func=AF.Square, accum_out=ss[:, 0:1]),
                 reads=[xkey], writes=["n_sq", "n_ss"])
            P.op("act", lambda: A.activation(out=ss[:, 1:2], in_=ss[:, 0:1], func=AF.Sqrt, bias=eps_ap, scale=1.0 / D),
                 reads=["n_ss", "cst"], writes=["n_ss1"])
            P.op("dve", lambda: V.reciprocal(out=ss[:, 2:3], in_=ss[:, 1:2]), reads=["n_ss1"], writes=["n_ss2"])
            P.op("dve", lambda: V.tensor_scalar(out=xs, in0=xt, scalar1=ss[:, 2:3], scalar2=None, op0=ALU.mult),
                 reads=[xkey, "n_ss2"], writes=["n_xs"])
            for kc in range(8):
                P.op("pe", lambda kc=kc: T.transpose(ptr[:, kc * 128:(kc + 1) * 128], xs[:, kc * 128:(kc + 1) * 128], ident_b),
                     reads=["n_xs", "cstb"], writes=[ptrkey])
            for kc in range(8):
                if kc % 2 == 0:
                    P.op("act", lambda kc=kc: A.activation(out=dst_fn(kc), in_=ptr[:, kc * 128:(kc + 1) * 128], func=AF.Identity,
                                                           scale=mod[:, gmc + kc:gmc + kc + 1], bias=mod[:, shc + kc:shc + kc + 1]),
                         reads=[ptrkey, "mod"], writes=[dstkey])
                else:
                    P.op("dve", lambda kc=kc: V.tensor_scalar(out=dst_fn(kc), in0=ptr[:, kc * 128:(kc + 1) * 128],
                                                              scalar1=mod[:, gmc + kc:gmc + kc + 1], scalar2=mod[:, shc + kc:shc + kc + 1],
                                                              op0=ALU.mult, op1=ALU.add),
                         reads=[ptrkey, "mod"], writes=[dstkey])

        with contextlib.ExitStack() as s2:
            hT = sb(s2, "hT", [128, 8, SEG], BF16)
            hcT = sb(s2, "hcT", [128, 8, CTX], BF16)
            lrT = [sb(s2, "lrT%d" % d, [17, SEG + CTX], BF16) for d in range(2)]
            lrw = sb(s2, "lrw", [17, 2, 512], BF16)
            wlr = sb(s2, "wlr", [128, 8, 32], BF16)
            QT = [sb(s2, "QT%d" % s, [128, NT, 2, 128], BF16) for s in range(2)]
            AT = [sb(s2, "AT%d" % s, [128, NT, 128], BF16) for s in range(2)]
            KH = [sb(s2, "KH%d" % s, [128, NT + NCT, 2, 128], BF16) for s in range(2)]
            VV = [sb(s2, "VV%d" % s, [128, NT + NCT, 256], BF16) for s in range(2)]
            DFB = [sb(s2, "DFB%d" % s, [128, NT + NCT, 2]) for s in range(2)]
            TOT = [sb(s2, "TOT%d" % s, [128, NT, 2]) for s in range(2)]
            XP = [sb(s2, "XP", [128, 520])] * 2
            XG = [sb(s2, "XG", [128, 4, 520])] * 2
            SC = [sb(s2, "SC", [128, 2, 256])] * 2
            CS = [sb(s2, "CS%d" % i, [128, 256]) for i in range(2)]
            ST = sb(s2, "ST", [128, 2, 256])
            STb = [sb(s2, "STb%d" % i, [128, 256], BF16) for i in range(2)]
            GS = sb(s2, "GS", [128, NT, 256], BF16)
            OGT = [sb(s2, "OGT", [128, 2, SEG], BF16)] * 2
            WQKV = [sb(s2, "WQKV%d" % s, [128, 8, 512], BF16) for s in range(2)]
            WGt = [sb(s2, "WG%d" % s, [128, 8, 256], BF16) for s in range(2)]
            esp = sb(s2, "esp", [128, 256])
            sp_hi = sb(s2, "sp_hi", [128, 256], BF16)
            sp_lo = sb(s2, "sp_lo", [128, 256], BF16)
            FA = [sb(s2, "FA%d" % i, [128, 6, 128]) for i in range(2)]
            QK4 = sb(s2, "QK4", [128, 4, 128], BF16)
            KTt = sb(s2, "KTt", [128, 2, 128], BF16)
            tAB = sb(s2, "tAB", [128, 256])
            qr = sb(s2, "qr", [128, 2, 128])
            rt1 = sb(s2, "rt1", [128, 2, 128])
            rt2 = sb(s2, "rt2", [128, 2, 128])
            sgt = [sb(s2, "sgt%d" % i, [128, 256]) for i in range(2)]
            ogt = [sb(s2, "ogt%d" % i, [128, 256], BF16) for i in range(2)]
            otmp = [sb(s2, "otmp%d" % i, [128, 256]) for i in range(2)]
            st8 = sb(s2, "st8", [128, 16])
            dpr = sb(s2, "dpr", [128, 8])
            xtmp = sb(s2, "xtmp", [128, 256])
            B = [pbank(s2, "B%d" % i) for i in range(4)]
            Bt = pbank(s2, "Bt", BF16)
            B5 = pbank(s2, "B5")
            B6 = pbank(s2, "B6")
            B7 = pbank(s2, "B7")
            print("phase2 sbuf remaining", nc.sbuf_bytes_remaining)

            xgf = XG[0][:].rearrange("p a b -> p (a b)")
            gsf = GS[:].rearrange("p a b -> p (a b)")
            xt2 = [xgf[:, 0:D], xgf[:, D:2 * D]]
            nsc = {"sq": gsf[:, 0:D], "ss": st8[:, 0:4], "xs": gsf[:, D:2 * D]}
            P.dma("pool", wlr[:], w_in_d[:, LRF:LRF + 32].rearrange("(kc p) c -> p kc c", p=128), writes=["wlr"])
            P.dma("pool", lrw[:], lrw_d, writes=["lrw"])
            for n in range(NT + NCT):
                xt = xt2[n % 2]
                xk = "xt%d" % (n % 2)
                if n < NT:
                    P.dma("sp", xt, x_d[n * 128:(n + 1) * 128, :], writes=[xk])
                    norm_T(xt, xk, Bt, "Bt", lambda kc, n=n: hT[:, kc, n * 128:(n + 1) * 128], "hT", M_GM1, M_SH1, nsc)
                else:
                    m = n - NT
                    P.dma("sp", xt, ctx_d[m * 128:(m + 1) * 128, :], writes=[xk])
                    norm_T(xt, xk, Bt, "Bt", lambda kc, m=m: hcT[:, kc, m * 128:(m + 1) * 128], "hcT", M_CGM1, M_CSH1, nsc)
            P.barrier()
            ck(1)
            for d in range(2):
                P.op("dve", lambda d=d: V.memset(lrT[d][:], 1.0), writes=["lrT%d" % d])
            for blk in range(5):
                src = hT if blk < 4 else hcT
                w = 512 if blk < 4 else CTX
                o0 = blk * 512 if blk < 4 else 0
                for d in range(2):
                    for kc in range(8):
                        P.op("pe", lambda d=d, kc=kc, src=src, o0=o0, w=w: T.matmul(
                            B[d][0:16, 0:w], wlr[:, kc, d * 16:(d + 1) * 16], src[:, kc, o0:o0 + w],
                            start=(kc == 0), stop=(kc == 7)), reads=["wlr", "hT", "hcT"], writes=["B%d" % d])
                    P.op("act", lambda d=d, blk=blk, w=w: A.copy(out=lrT[d][0:16, blk * 512:blk * 512 + w], in_=B[d][0:16, 0:w]),
                         reads=["B%d" % d], writes=["lrT%d" % d])

            ck(11)
            def load_weights(u, which):
                s = u % 2
                isg = u < 4
                h = u % 4
                qo, ko, vo, go = (GQ, GK, GV, GG) if isg else (RQ, RK, RV, RG)
                r = lambda c0, w: w_in_d[:, c0:c0 + w].rearrange("(kc p) c -> p kc c", p=128)
                if which == "qkv":
                    P.dma("pool", WQKV[s][:, :, 0:128], r(qo + h * 128, 128), writes=["WQKV%d" % s])
                    P.dma("pool", WQKV[s][:, :, 128:256], r(ko + h * 128, 128), writes=["WQKV%d" % s])
                    P.dma("pool", WQKV[s][:, :, 256:512], r(vo + h * 256, 256), writes=["WQKV%d" % s])
                else:
                    P.dma("pool", WGt[s][:], r(go + h * 256, 256), writes=["WG%d" % s])

            def passA(u):
                s = u % 2
                isg = u < 4
                h = u % 4
                qs = 128.0 ** -0.5 if isg else 1.0
                NA = NT + NCT

                def geom(n):
                    lat = n < NT
                    src = hT if lat else hcT
                    t0 = n * 128 if lat else (n - NT) * 128
                    lcol = n * 128 if lat else SEG + (n - NT) * 128
                    return lat, src, t0, lcol

                def stage1(n):
                    lat, src, t0, lcol = geom(n)
                    pp = n % 2
                    pq = B[pp]
                    pqk = "B%d" % pp
                    steps = []

                    def a0():
                        for kc in range(8):
                            P.op("pe", lambda kc=kc: T.matmul(pq[:, 0:512], src[:, kc, t0:t0 + 128], WQKV[s][:, kc, :], start=(kc == 0), stop=(kc == 7)),
                                 reads=["hT", "hcT", "WQKV%d" % s], writes=[pqk])
                        if isg:
                            for d in range(2):
                                P.op("pe", lambda d=d: T.matmul(B[2][:, d * 128:(d + 1) * 128], lrT[d][0:17, lcol:lcol + 128], lrw[0:17, d, h * 128:(h + 1) * 128],
                                                                start=True, stop=True), reads=["lrT%d" % d, "lrw"], writes=["B2"])
                        elif lat:
                            P.dma("sp", CS[pp][:], cst_d[:, C_CS + n * 256:C_CS + (n + 1) * 256], writes=["CS%d" % pp])

                    def a1():
                        P.op("act", lambda: A.copy(out=VV[s][:, n, :], in_=pq[:, 256:512]), reads=[pqk], writes=["VV%d_%d" % (s, n)])
                        if isg:
                            P.op("act", lambda: A.activation(out=esp[:], in_=B[2][:, 0:256], func=AF.Exp, scale=-1.0), reads=["B2"], writes=["esp"])
                            P.op("act", lambda: A.activation(out=esp[:], in_=esp[:], func=AF.Ln, bias=1.0), reads=["esp"], writes=["esp"])

                    def a2():
                        P.op("dve", lambda: V.tensor_copy(out=sp_hi[:], in_=esp[:]), reads=["esp"], writes=["sp_hi"])
                        P.op("dve", lambda: V.tensor_tensor(out=sp_lo[:], in0=esp[:], in1=sp_hi[:], op=ALU.subtract), reads=["esp", "sp_hi"], writes=["sp_lo"])

                    def a3():
                        for ci, (mo, d) in enumerate(((C_LE, 0), (C_GT, 0), (C_GE, 1), (C_LT, 1))):
                            for pi, part in enumerate((sp_hi, sp_lo)):
                                P.op("pe", lambda ci=ci, mo=mo, d=d, part=part, pi=pi: T.matmul(
                                    B[3][:, ci * 128:(ci + 1) * 128], cstb[:, mo:mo + 128], part[:, d * 128:(d + 1) * 128],
                                    start=(pi == 0), stop=(pi == 1)), reads=["cstb", "sp_hi", "sp_lo"], writes=["B3"])
                        for d in range(2):
                            for pi, part in enumerate((sp_hi, sp_lo)):
                                P.op("pe", lambda d=d, part=part, pi=pi: T.matmul(
                                    B[2][:, 256 + 2 * d:258 + 2 * d], part[:, d * 128:(d + 1) * 128], cstb[:, C_ONE:C_ONE + 2],
                                    start=(pi == 0), stop=(pi == 1)), reads=["cstb", "sp_hi", "sp_lo"], writes=["B2"])

                    def a4():
                        cumv = B[3][:, 0:512].rearrange("p (a b) -> p a b", a=4)
                        fa = FA[pp]
                        P.op("act", lambda: A.activation(out=fa[:, 0:3:2, :], in_=cumv[:, 0:3:2, :], func=AF.Exp, scale=-1.0 / TAU, bias=lnqs_ap),
                             reads=["B3", "cst"], writes=["FA%d" % pp])
                        P.op("act", lambda: A.activation(out=fa[:, 4:6, :], in_=cumv[:, 1:4:2, :], func=AF.Exp, scale=-1.0 / TAU),
                             reads=["B3"], writes=["FA%d" % pp])
                        P.op("act", lambda: A.activation(out=fa[:, 1:4:2, :], in_=cumv[:, 0:3:2, :], func=AF.Exp, scale=1.0 / TAU),
                             reads=["B3"], writes=["FA%d" % pp])
                        P.op("act", lambda: A.activation(out=DFB[s][:, n, :], in_=B[2][:, 256:260:2], func=AF.Exp, scale=-1.0 / TAU),
                             reads=["B2"], writes=["DFB%d" % s])
                        if lat:
                            P.op("dve", lambda: V.tensor_copy(out=TOT[s][:, n, :], in_=B[2][:, 256:260:2]), reads=["B2"], writes=["TOT%d" % s])

                    steps = [a0, a1] + ([a2, a3, a4] if isg else [])
                    return steps

                def stage2(n):
                    lat, src, t0, lcol = geom(n)
                    pp = n % 2
                    pq = B[pp]
                    pqk = "B%d" % pp
                    qps = pq[:, 0:128]
                    kps = pq[:, 128:256]
                    steps = []
                    if isg:
                        def b0():
                            fa = FA[pp]
                            qk = pq[:, 0:256].rearrange("p (j w) -> p j w", j=2)
                            if lat:
                                P.op("dve", lambda: V.tensor_tensor(out=QK4[:].rearrange("p (r j) w -> p r j w", r=2), in0=qk.unsqueeze(1).to_broadcast([128, 2, 2, 128]),
                                                                    in1=fa[:, 0:4, :].rearrange("p (r j) w -> p r j w", r=2), op=ALU.mult),
                                     reads=[pqk, "FA%d" % pp], writes=["QK4"])
                            P.op("dve", lambda: V.tensor_tensor(out=KH[s][:, n, :, :], in0=kps.unsqueeze(1).to_broadcast([128, 2, 128]), in1=fa[:, 4:6, :], op=ALU.mult),
                                 reads=[pqk, "FA%d" % pp], writes=["KH%d_%d" % (s, n)])
                        steps.append(b0)
                    else:
                        rf = lambda i: cst[:, C_RF + h * 6 + i:C_RF + h * 6 + i + 1]
                        rfv = cst[:, C_RF + h * 6:C_RF + h * 6 + 6].rearrange("p (r c) -> p r c", r=2)
                        if lat:
                            csk = "CS%d" % pp
                            cosn = CS[pp][:, 0:128]
                            sinn = CS[pp][:, 128:256]
                            qk = pq[:, 0:256]

                            def rope():
                                P.op("dve", lambda: V.tensor_tensor(out=rt1[:], in0=qk.rearrange("p (j w) -> p j w", j=2), in1=cosn.unsqueeze(1).to_broadcast([128, 2, 128]), op=ALU.mult),
                                     reads=[pqk, csk], writes=["rt1"])
                                pv = qk.rearrange("p (j h s w) -> p (j h) s w", j=2, h=2, s=2, w=32)
                                sv = sinn.rearrange("p (h s w) -> p h s w", h=2, s=2, w=32)
                                ov = rt2[:].rearrange("p j (h s w) -> p (j h) s w", h=2, s=2, w=32)
                                for sidx in range(2):
                                    for j in range(2):
                                        P.op("dve", lambda sidx=sidx, j=j: V.tensor_tensor(
                                            out=ov[:, 2 * j:2 * j + 2, sidx, :], in0=pv[:, 2 * j:2 * j + 2, 1 - sidx, :], in1=sv[:, :, sidx, :], op=ALU.mult),
                                            reads=[pqk, csk], writes=["rt2"])
                                P.op("dve", lambda: V.tensor_tensor(out=qr[:], in0=rt1[:], in1=rt2[:], op=ALU.add), reads=["rt1", "rt2"], writes=["qr"])
                            steps.append(rope)
                            qkr = qr[:]
                            ka = qr[:, 1, :]
                            rk = ["qr"]
                        else:
                            qkr = None
                            ka = kps
                            rk = [pqk]

                        def b2r():
                            if lat:
                                P.op("dve", lambda: V.tensor_tensor(out=QK4[:].rearrange("p (r j) w -> p r j w", r=2), in0=qkr.unsqueeze(1).to_broadcast([128, 2, 2, 128]),
                                                                    in1=rfv[:, :, 0:2].unsqueeze(3).to_broadcast([128, 2, 2, 128]), op=ALU.mult),
                                     reads=rk + ["cst"], writes=["QK4"])
                            P.op("dve", lambda: V.tensor_tensor(out=KH[s][:, n, :, :], in0=ka.unsqueeze(1).to_broadcast([128, 2, 128]),
                                                                in1=rfv[:, :, 2:3].to_broadcast([128, 2, 128]), op=ALU.mult),
                                 reads=rk + ["cst"], writes=["KH%d_%d" % (s, n)])
                        steps.append(b2r)
                    if not lat:
                        return steps

                    def b1():
                        for i in range(4):
                            P.op("pe", lambda i=i: T.transpose(Bt[:, i * 128:(i + 1) * 128], QK4[:, i, :], ident_b), reads=["QK4", "cstb"], writes=["Bt"])

                    def b2():
                        btv = Bt[:, 0:512].rearrange("p (a b) -> p a b", a=4)
                        P.op("act", lambda: A.copy(out=QT[s][:, n, :, :], in_=btv[:, 0:3:2, :]), reads=["Bt"], writes=["QT%d_%d" % (s, n)])
                        P.op("dve", lambda: V.tensor_copy(out=KTt[:], in_=btv[:, 1:4:2, :]), reads=["Bt"], writes=["KTt"])

                    def b3():
                        for d in range(2):
                            P.op("pe", lambda d=d: T.matmul(B5[:, d * 128:(d + 1) * 128], KTt[:, d, :], QT[s][:, n, d, :], start=True, stop=True),
                                 reads=["KTt", "QT%d_%d" % (s, n)], writes=["B5"])

                    def b4():
                        P.op("dve", lambda: V.tensor_tensor(out=tAB[:], in0=B5[:, 0:256], in1=cst[:, C_LE:C_LE + 256], op=ALU.mult), reads=["B5", "cst"], writes=["tAB"])
                        P.op("dve", lambda: V.tensor_tensor(out=AT[s][:, n, :], in0=tAB[:, 0:128], in1=tAB[:, 128:256], op=ALU.add), reads=["tAB"], writes=["AT%d_%d" % (s, n)])
                    steps += [b1, b2, b3, b4]
                    return steps

                if not isg:
                    P.op("dve", lambda: V.tensor_copy(out=DFB[s][:], in_=cst[:, C_RD + 2 * h:C_RD + 2 * h + 2].unsqueeze(1).to_broadcast([128, NT + NCT, 2])),
                         reads=["cst"], writes=["DFB%d" % s])
                for st in stage1(0):
                    st()
                yield
                for n in range(NA):
                    sa = stage1(n + 1) if n + 1 < NA else []
                    sb_ = stage2(n)
                    for i in range(max(len(sa), len(sb_))):
                        if i < len(sa):
                            sa[i]()
                        if i < len(sb_):
                            sb_[i]()
                        yield

            sctr = [0]

            def state_step(s, n, d, stv, stkey, first):
                bi = 2 + (sctr[0] % 2)
                sctr[0] += 1
                bk = B[bi]
                bkey = "B%d" % bi
                P.op("pe", lambda: T.matmul(bk[:, 0:256], KH[s][:, n, d, :], VV[s][:, n, :], start=True, stop=True),
                     reads=["KH%d_%d" % (s, n), "VV%d_%d" % (s, n)], writes=[bkey])
                if first:
                    P.op("dve", lambda: V.tensor_copy(out=stv, in_=bk[:, 0:256]), reads=[bkey], writes=[stkey])
                else:
                    P.op("dve", lambda: V.scalar_tensor_tensor(out=stv, in0=stv, scalar=DFB[s][:, n, d:d + 1], in1=bk[:, 0:256], op0=ALU.mult, op1=ALU.add),
                         reads=[bkey, stkey, "DFB%d" % s], writes=[stkey])

            def passL(u):
                s = u % 2
                isg = u < 4
                h = u % 4
                state_step(s, NT, 0, SC[s][:, 0, :], "SC_0", True)
                state_step(s, NT + 1, 1, SC[s][:, 1, :], "SC_1", True)
                state_step(s, NT + 1, 0, SC[s][:, 0, :], "SC_0", False)
                state_step(s, NT, 1, SC[s][:, 1, :], "SC_1", False)
                for k in range(NT):
                    state_step(s, k, 0, XP[s][:, 0:256], "XP_0", k == 0)
                    state_step(s, NT - 1 - k, 1, XP[s][:, 256:512], "XP_1", k == 0)
                if isg:
                    P.op("dve", lambda: V.tensor_reduce(out=st8[:, 0:2], in_=TOT[s][:].rearrange("p n d -> p d n"), axis=AX.X, op=ALU.add),
                         reads=["TOT%d" % s], writes=["st8"])
                    P.op("act", lambda: A.activation(out=XP[s][:, 512:514], in_=st8[:, 0:2], func=AF.Exp, scale=-1.0 / TAU), reads=["st8"], writes=["XP_2"])
                else:
                    P.op("dve", lambda: V.tensor_copy(out=XP[s][:, 512:514], in_=cst[:, C_RDT + 2 * h:C_RDT + 2 * h + 2]), reads=["cst"], writes=["XP_2"])
                P.op("dve", lambda: V.memset(XP[s][:, 514:520], 0.0), writes=["XP_3"])
                P.dma("sp", cin_d[u].ap(), XP[s][:], reads=["XP_0", "XP_1", "XP_2", "XP_3"], writes=["cin%d" % u])
                P.collective("AllGather", ALU.bypass, [[0, 1, 2, 3], [4, 5, 6, 7]], [cin_d[u].ap().opt()], [cout_d[u].ap().opt()],
                             reads=["cin%d" % u], writes=["cout%d" % u])
                P.dma("pool", XG[s][:], cout_d[u].ap().rearrange("(r p) c -> p r c", p=128), reads=["cout%d" % u], writes=["XG"])

            def passBC(u):
                s = u % 2
                isg = u < 4
                h = u % 4
                for d in range(2):
                    P.op("dve", lambda d=d: V.tensor_copy(out=ST[:, d, :], in_=SC[s][:, d, :]), reads=["SC_%d" % d], writes=["ST%d" % d])
                    order = range(4) if d == 0 else range(3, -1, -1)
                    for i in order:
                        mcol = cst[:, C_CM + d * 4 + i:C_CM + d * 4 + i + 1]
                        P.op("dve", lambda d=d, i=i, mcol=mcol: V.tensor_scalar(out=dpr[:, 2 * d:2 * d + 1], in0=XG[s][:, i, 512 + d:513 + d], scalar1=-1.0, scalar2=mcol, op0=ALU.add, op1=ALU.mult),
                             reads=["XG", "cst"], writes=["dpr%d" % d])
                        P.op("dve", lambda d=d: V.tensor_scalar(out=dpr[:, 2 * d + 1:2 * d + 2], in0=dpr[:, 2 * d:2 * d + 1], scalar1=1.0, scalar2=None, op0=ALU.add), reads=["dpr%d" % d], writes=["dprb%d" % d])
                        P.op("dve", lambda d=d, i=i, mcol=mcol: V.tensor_scalar(out=xtmp[:], in0=XG[s][:, i, d * 256:(d + 1) * 256], scalar1=mcol, scalar2=None, op0=ALU.mult),
                             reads=["XG", "cst"], writes=["xtmp"])
                        P.op("dve", lambda d=d: V.scalar_tensor_tensor(out=ST[:, d, :], in0=ST[:, d, :], scalar=dpr[:, 2 * d + 1:2 * d + 2], in1=xtmp[:], op0=ALU.mult, op1=ALU.add),
                             reads=["dprb%d" % d, "xtmp", "ST%d" % d], writes=["ST%d" % d])
                        yield
                for n in range(NT - 1, -1, -1):
                    P.op("act", lambda n=n: A.copy(out=GS[:, n, :], in_=ST[:, 1, :]), reads=["ST1"], writes=["GS_%d" % n])
                    if n > 0:
                        state_step(s, n, 1, ST[:, 1, :], "ST1", False)
                    yield
                BO = [B6, B7]

                def mm(n):
                    pp = n % 2
                    bo = BO[pp]
                    bok = "B%d" % (6 + pp)
                    P.op("act", lambda: A.copy(out=STb[pp][:], in_=ST[:, 0, :]), reads=["ST0"], writes=["STb%d" % pp])
                    P.op("pe", lambda: T.matmul(bo[:, 0:256], AT[s][:, n, :], VV[s][:, n, :], start=True, stop=False),
                         reads=["AT%d_%d" % (s, n), "VV%d_%d" % (s, n)], writes=[bok])
                    P.op("pe", lambda: T.matmul(bo[:, 0:256], QT[s][:, n, 0, :], STb[pp][:], start=False, stop=False),
                         reads=["QT%d_%d" % (s, n), "STb%d" % pp], writes=[bok])
                    P.op("pe", lambda: T.matmul(bo[:, 0:256], QT[s][:, n, 1, :], GS[:, n, :], start=False, stop=True),
                         reads=["QT%d_%d" % (s, n), "GS_%d" % n], writes=[bok])
                    for kc in range(8):
                        P.op("pe", lambda kc=kc: T.matmul(bo[:, 256:512], hT[:, kc, n * 128:(n + 1) * 128], WGt[s][:, kc, :], start=(kc == 0), stop=(kc == 7)),
                             reads=["hT", "WG%d" % s], writes=[bok])
                    if n < NT - 1:
                        state_step(s, n, 0, ST[:, 0, :], "ST0", False)

                def epi(n):
                    pp = n % 2
                    bo = BO[pp]
                    bok = "B%d" % (6 + pp)
                    sg_, og_, ot_ = sgt[pp], ogt[pp], otmp[pp]
                    P.op("act", lambda: A.activation(out=sg_[:], in_=bo[:, 256:512], func=AF.Silu), reads=[bok], writes=["sgt%d" % pp])
                    if isg:
                        P.op("act", lambda: A.activation(out=ot_[:], in_=bo[:, 0:256], func=AF.Square, accum_out=st8[:, 4:5]), reads=[bok], writes=["otmp%d" % pp, "st8a"])
                        P.op("act", lambda: A.activation(out=st8[:, 5:6], in_=st8[:, 4:5], func=AF.Sqrt, bias=eps_ap, scale=1.0 / 256), reads=["st8a", "cst"], writes=["st8b"])
                        P.op("dve", lambda: V.reciprocal(out=st8[:, 6:7], in_=st8[:, 5:6]), reads=["st8b"], writes=["st8c"])
                        P.op("dve", lambda: V.scalar_tensor_tensor(out=og_[:], in0=bo[:, 0:256], scalar=st8[:, 6:7], in1=sg_[:], op0=ALU.mult, op1=ALU.mult),
                             reads=[bok, "st8c", "sgt%d" % pp], writes=["ogt%d" % pp])
                    else:
                        P.op("dve", lambda: V.bn_stats(out=st8[:, 8:14], in_=bo[:, 0:256]), reads=[bok], writes=["st8s"])
                        P.op("dve", lambda: V.bn_aggr(out=st8[:, 14:16], in_=st8[:, 8:14]), reads=["st8s"], writes=["st8m"])
                        P.op("act", lambda: A.activation(out=st8[:, 5:6], in_=st8[:, 15:16], func=AF.Sqrt, bias=eps_ap, scale=1.0), reads=["st8m", "cst"], writes=["st8b"])
                        P.op("dve", lambda: V.reciprocal(out=st8[:, 6:7], in_=st8[:, 5:6]), reads=["st8b"], writes=["st8c"])
                        P.op("dve", lambda: V.tensor_scalar(out=ot_[:], in0=bo[:, 0:256], scalar1=st8[:, 14:15], scalar2=st8[:, 6:7], op0=ALU.subtract, op1=ALU.mult),
                             reads=[bok, "st8m", "st8c"], writes=["otmp%d" % pp])
                        P.op("dve", lambda: V.tensor_tensor(out=og_[:], in0=ot_[:], in1=sg_[:], op=ALU.mult), reads=["otmp%d" % pp, "sgt%d" % pp], writes=["ogt%d" % pp])
                    for c in range(2):
                        P.op("pe", lambda c=c: T.transpose(Bt[:, 512 + c * 128:512 + (c + 1) * 128], og_[:, c * 128:(c + 1) * 128], ident_b), reads=["ogt%d" % pp, "cstb"], writes=["Bt"])
                    P.op("act", lambda: A.copy(out=OGT[s][:, :, n * 128:(n + 1) * 128], in_=Bt[:, 512:768].rearrange("p (c t) -> p c t", c=2)),
                         reads=["Bt"], writes=["OGT"])

                mm(0)
                yield
                for n in range(NT):
                    if n + 1 < NT:
                        mm(n + 1)
                        yield
                    epi(n)
                    yield
                P.dma("sp", ogT_d.ap()[u * 256:(u + 1) * 256, :].rearrange("(c p) t -> p c t", p=128), OGT[s][:], reads=["OGT"], writes=["ogT_d"])


            load_weights(0, "qkv")
            load_weights(0, "g")
            for u in range(8):
                if u + 1 < 8:
                    load_weights(u + 1, "qkv")
                ga = passA(u)
                gb = passBC(u - 1) if u >= 1 else iter(())
                da = db = False
                if u < 4:
                    for _ in ga:
                        pass
                    for _ in gb:
                        pass
                    da = db = True
                while not (da and db):
                    if not da:
                        try:
                            next(ga)
                        except StopIteration:
                            da = True
                    if not db:
                        try:
                            next(gb)
                        except StopIteration:
                            db = True
                if u == 0:
                    ck(2)
                if u + 1 < 8:
                    load_weights(u + 1, "g")
                passL(u)
                if u == 0:
                    ck(3)
                if u == 1:
                    ck(4)
            for _ in passBC(7):
                pass
            if "ogt_last" in dbg_out:
                pass
        P.barrier()
        ck(5)

        with contextlib.ExitStack() as s3:
            YH = sb(s3, "YH", [128, 8, SEG], BF16)
            r = lambda ap_: ap_.rearrange("(kc p) c -> p kc c", p=128)
            with contextlib.ExitStack() as s3a:
                nsc = {"sq": sb(s3a, "m_sq", [128, D], BF16)[:], "ss": sb(s3a, "m_ss", [128, 4])[:], "xs": sb(s3a, "m_xs", [128, D], BF16)[:]}
                WM = sb(s3a, "WM", [128, 8, 2048], BF16)
                WBG = sb(s3a, "WBG", [128, 8, D], BF16)
                WBR = sb(s3a, "WBR", [128, 8, D], BF16)
                OGB = [sb(s3a, "OGB%d" % i, [128, 16, 256], BF16) for i in range(2)]
                hTb = sb(s3a, "hTb", [128, 8, 256], BF16)
                xt3 = [sb(s3a, "x3_%d" % i, [128, D]) for i in range(2)]
                sg2 = [sb(s3a, "sg2_%d" % i, [128, 2, 256]) for i in range(2)]
                y12 = sb(s3a, "y12", [128, 2, 256])
                PM = [pbank(s3a, "PM%d" % i) for i in range(2)]
                PB = [pbank(s3a, "PB%d" % i) for i in range(2)]
                PT = pbank(s3a, "PT", BF16)
                P.dma("pool", WM[:], r(w_in_d[:, MG:MG + 2048]), writes=["WM"])
                P.dma("pool", WBG[:], r(wbg_d), writes=["WBG"])
                P.dma("pool", WBR[:], r(wbr_d), writes=["WBR"])
                for fc in range(8):
                    P.op("dve", lambda fc=fc: V.tensor_scalar(out=WBG[:, fc, :], in0=WBG[:, fc, :], scalar1=vec[:, VC_GN + fc % 2:VC_GN + fc % 2 + 1], scalar2=None, op0=ALU.mult),
                         reads=["WBG", "vec"], writes=["WBG"])
                for blk in range(8):
                    ob = OGB[blk % 2]
                    obk = "OGB%d" % (blk % 2)
                    P.dma("sp", ob[:], ogT_d.ap()[:, blk * 256:(blk + 1) * 256].rearrange("(c p) t -> p c t", p=128), writes=[obk])
                    for tt in range(2):
                        n = blk * 2 + tt
                        xt = xt3[n % 2]
                        xk = "x3_%d" % (n % 2)
                        P.dma("sp", xt[:], x_d[n * 128:(n + 1) * 128, :], writes=[xk])
                        norm_T(xt[:], xk, PT, "PT", lambda kc, tt=tt: hTb[:, kc, tt * 128:(tt + 1) * 128], "hTb", M_GM1, M_SH1, nsc)
                    for nch in range(8):
                        pm = PM[nch % 2]
                        pb = PB[nch % 2]
                        pmk = "PM%d" % (nch % 2)
                        pbk = "PB%d" % (nch % 2)
                        sg = sg2[nch % 2]
                        sgk = "sg2_%d" % (nch % 2)
                        for br_ in range(2):
                            for kc in range(8):
                                P.op("pe", lambda br_=br_, kc=kc, pm=pm, nch=nch: T.matmul(
                                    pm[:, br_ * 256:(br_ + 1) * 256], WM[:, kc, br_ * 1024 + nch * 128:br_ * 1024 + (nch + 1) * 128], hTb[:, kc, :],
                                    start=(kc == 0), stop=(kc == 7)), reads=["WM", "hTb"], writes=[pmk])
                        for br_ in range(2):
                            wb = WBG if br_ == 0 else WBR
                            for fc in range(8):
                                P.op("pe", lambda br_=br_, fc=fc, pb=pb, wb=wb, nch=nch, ob=ob: T.matmul(
                                    pb[:, br_ * 256:(br_ + 1) * 256], wb[:, fc, nch * 128:(nch + 1) * 128], ob[:, br_ * 8 + fc, :],
                                    start=(fc == 0), stop=(fc == 7)), reads=["WBG", "WBR", obk], writes=[pbk])
                        P.op("act", lambda pm=pm, sg=sg: A.activation(out=sg[:].rearrange("p a b -> p (a b)"), in_=pm[:, 0:512], func=AF.Sigmoid), reads=[pmk], writes=[sgk])
                        P.op("dve", lambda pb=pb, sg=sg: V.tensor_tensor(out=y12[:].rearrange("p a b -> p (a b)"), in0=pb[:, 0:512], in1=sg[:].rearrange("p a b -> p (a b)"), op=ALU.mult),
                             reads=[pbk, sgk], writes=["y12"])
                        P.op("dve", lambda nch=nch, blk=blk: V.tensor_tensor(out=YH[:, nch, blk * 256:(blk + 1) * 256], in0=y12[:, 0, :], in1=y12[:, 1, :], op=ALU.add),
                             reads=["y12"], writes=["YH_%d" % blk])
            P.barrier()
            ck(6)
            HL = sb(s3, "HL", [128, NT, D])
            with contextlib.ExitStack() as s3b:
                WO = sb(s3b, "WO", [128, 8, D], BF16)
                xt4 = [sb(s3b, "x4_%d" % i, [128, D]) for i in range(2)]
                otmp3 = sb(s3b, "otmp3", [128, D])
                PO = [pbank(s3b, "PO%d" % i) for i in range(4)]
                P.dma("pool", WO[:], r(wout_d), writes=["WO"])
                for n in range(NT):
                    xt = xt4[n % 2]
                    xk = "x4_%d" % (n % 2)
                    P.dma("sp", xt[:], x_d[n * 128:(n + 1) * 128, :], writes=[xk])
                    for half in range(2):
                        po = PO[(n % 2) * 2 + half]
                        pok = "PO%d" % ((n % 2) * 2 + half)
                        for kc in range(8):
                            P.op("pe", lambda kc=kc, po=po, half=half, n=n: T.matmul(
                                po[:, 0:512], YH[:, kc, n * 128:(n + 1) * 128], WO[:, kc, half * 512:(half + 1) * 512],
                                start=(kc == 0), stop=(kc == 7)), reads=["YH_%d" % (n // 2), "WO"], writes=[pok])
                        P.op("dve", lambda po=po, half=half: V.tensor_tensor(out=otmp3[:, half * 512:(half + 1) * 512], in0=po[:, 0:512], in1=g1bc[:, half * 512:(half + 1) * 512], op=ALU.mult),
                             reads=[pok, "g1bc"], writes=["otmp3_%d" % half])
                        P.op("dve", lambda n=n, half=half, xt=xt: V.tensor_tensor(out=HL[:, n, half * 512:(half + 1) * 512], in0=otmp3[:, half * 512:(half + 1) * 512], in1=xt[:, half * 512:(half + 1) * 512], op=ALU.add),
                             reads=["otmp3_%d" % half, xk], writes=["HL_%d" % n])
            P.barrier()
            ck(7)
            if "hl" in dbg_out:
                P.dma("sp", dbg_out["hl"].rearrange("(n p) d -> p n d", p=128), HL[:], reads=["HL_%d" % n for n in range(NT)], writes=["dbg_hl"])

            with contextlib.ExitStack() as s4:
                H2T = YH
                WR = sb(s4, "WR", [128, 8, 36])
                brow = sb(s4, "brow", [1, 36])
                WTs = sb(s4, "WTs", [128, NT, 32])
                h2f = sb(s4, "h2f", [128, 8, 128])
                xsf = sb(s4, "xsf", [128, D])
                sqf = sb(s4, "sqf", [128, D], BF16)
                L = sb(s4, "L", [128, 36])
                rs = sb(s4, "rs", [128, 16])
                r4 = sb(s4, "r4", [128, 8])
                elm = sb(s4, "elm", [128, 32])
                ex = sb(s4, "ex", [128, 32])
                sel = sb(s4, "sel", [128, 32])
                top8 = sb(s4, "top8", [128, 8])
                WGU = [sb(s4, "WGU%d" % i, [128, 2, 8, 512], BF16) for i in range(2)]
                WDn = [sb(s4, "WDn%d" % i, [128, 2, 2, D], BF16) for i in range(2)]
                sgm = [sb(s4, "sgm%d" % i, [128, 256]) for i in range(4)]
                hid = [sb(s4, "hid%d" % i, [128, 256], BF16) for i in range(4)]
                HIDT = [sb(s4, "HIDT%d" % i, [128, 2, 2, 128], BF16) for i in range(2)]
                yo = sb(s4, "yo", [128, D])
                PG = [pbank(s4, "PG%d" % i) for i in range(4)]
                PD = [pbank(s4, "PD%d" % i) for i in range(2)]
                PX = pbank(s4, "PX", BF16)
                PF = pbank(s4, "PF")
                P.dma("sp", WR[:], wr_d.rearrange("(kc p) c -> p kc c", p=128), writes=["WR"])
                P.dma("sp", brow[:], br_d, writes=["brow"])

                def load_experts(j):
                    bi = j % 2
                    for e in range(2):
                        ee = 2 * j + e
                        P.dma("pool", WGU[bi][:, e, :, 0:256], weg_d[ee].rearrange("(kc p) c -> p kc c", p=128), writes=["WGU%d" % bi])
                        P.dma("pool", WGU[bi][:, e, :, 256:512], weu_d[ee].rearrange("(kc p) c -> p kc c", p=128), writes=["WGU%d" % bi])
                        P.dma("pool", WDn[bi][:, e, :, :], wed_d[ee].rearrange("(fc p) c -> p fc c", p=128), writes=["WDn%d" % bi])

                load_experts(0)
                for n in range(NT):
                    P.op("act", lambda n=n: A.activation(out=sqf[:], in_=HL[:, n, :], func=AF.Square, accum_out=rs[:, 0:1]), reads=["HL_%d" % n], writes=["sqf", "rs0"])
                    P.op("act", lambda: A.activation(out=rs[:, 1:2], in_=rs[:, 0:1], func=AF.Sqrt, bias=eps_ap, scale=1.0 / D), reads=["rs0", "cst"], writes=["rs1"])
                    P.op("dve", lambda: V.reciprocal(out=rs[:, 2:3], in_=rs[:, 1:2]), reads=["rs1"], writes=["rs2"])
                    P.op("dve", lambda n=n: V.tensor_scalar(out=xsf[:], in0=HL[:, n, :], scalar1=rs[:, 2:3], scalar2=None, op0=ALU.mult), reads=["HL_%d" % n, "rs2"], writes=["xsf"])
                    for kc in range(8):
                        pd = PD[kc // 4]
                        P.op("pe", lambda kc=kc, pd=pd: T.transpose(pd[:, (kc % 4) * 128:(kc % 4 + 1) * 128], xsf[:, kc * 128:(kc + 1) * 128], ident_f),
                             reads=["xsf", "cst"], writes=["PD%d" % (kc // 4)])
                    for kc in range(8):
                        pd = PD[kc // 4]
                        src = pd[:, (kc % 4) * 128:(kc % 4 + 1) * 128]
                        if kc % 2 == 0:
                            P.op("act", lambda kc=kc, src=src: A.activation(out=h2f[:, kc, :], in_=src, func=AF.Identity, scale=mod[:, M_GM2 + kc:M_GM2 + kc + 1], bias=mod[:, M_SH2 + kc:M_SH2 + kc + 1]),
                                 reads=["PD%d" % (kc // 4), "mod"], writes=["h2f"])
                        else:
                            P.op("dve", lambda kc=kc, src=src: V.tensor_scalar(out=h2f[:, kc, :], in0=src, scalar1=mod[:, M_GM2 + kc:M_GM2 + kc + 1], scalar2=mod[:, M_SH2 + kc:M_SH2 + kc + 1], op0=ALU.mult, op1=ALU.add),
                                 reads=["PD%d" % (kc // 4), "mod"], writes=["h2f"])
                    P.op("dve", lambda n=n: V.tensor_copy(out=H2T[:, :, n * 128:(n + 1) * 128], in_=h2f[:]), reads=["h2f"], writes=["H2T_%d" % n])
                    for kc in range(8):
                        P.op("pe", lambda kc=kc: T.matmul(PF[:, 0:36], h2f[:, kc, :], WR[:, kc, :], start=(kc == 0), stop=False), reads=["h2f", "WR"], writes=["PF"])
                    P.op("pe", lambda: T.matmul(PF[:, 0:36], cst[0:1, C_ONE:C_ONE + 128], brow[0:1, :], start=False, stop=True), reads=["cst", "brow"], writes=["PF"])
                    P.op("dve", lambda: V.tensor_copy(out=L[:], in_=PF[:, 0:36]), reads=["PF"], writes=["L"])
                    P.op("dve", lambda: V.tensor_reduce(out=rs[:, 4:5], in_=L[:, 0:4], axis=AX.X, op=ALU.max), reads=["L"], writes=["rs4"])
                    P.op("dve", lambda: V.tensor_scalar(out=r4[:, 0:4], in0=L[:, 0:4], scalar1=rs[:, 4:5], scalar2=None, op0=ALU.is_equal), reads=["L", "rs4"], writes=["r4a"])
                    P.op("dve", lambda: V.tensor_scalar(out=rs[:, 5:6], in0=rs[:, 4:5], scalar1=-1.0, scalar2=None, op0=ALU.mult), reads=["rs4"], writes=["rs5"])
                    P.op("act", lambda: A.activation(out=r4[:, 4:8], in_=L[:, 0:4], func=AF.Exp, bias=rs[:, 5:6], accum_out=rs[:, 6:7]), reads=["L", "rs5"], writes=["r4b", "rs6"])
                    P.op("dve", lambda: V.reciprocal(out=rs[:, 7:8], in_=rs[:, 6:7]), reads=["rs6"], writes=["rs7"])
                    P.op("dve", lambda: V.tensor_scalar(out=r4[:, 0:4], in0=r4[:, 0:4], scalar1=BIG, scalar2=-BIG, op0=ALU.mult, op1=ALU.add), reads=["r4a"], writes=["r4a"])
                    P.op("dve", lambda: V.tensor_tensor(out=elm[:].rearrange("p (g e) -> p g e", g=4), in0=L[:, 4:36].rearrange("p (g e) -> p g e", g=4),
                                                        in1=r4[:, 0:4].unsqueeze(2).to_broadcast([128, 4, 8]), op=ALU.add), reads=["L", "r4a"], writes=["elm"])
                    P.op("dve", lambda: V.max(out=top8[:], in_=elm[:]), reads=["elm"], writes=["top8"])
                    P.op("dve", lambda: V.tensor_scalar(out=sel[:], in0=elm[:], scalar1=top8[:, 1:2], scalar2=None, op0=ALU.is_ge), reads=["elm", "top8"], writes=["sel"])
                    P.op("dve", lambda: V.tensor_scalar(out=rs[:, 8:9], in0=top8[:, 0:1], scalar1=-1.0, scalar2=None, op0=ALU.mult), reads=["top8"], writes=["rs8"])
                    P.op("act", lambda: A.activation(out=ex[:], in_=elm[:], func=AF.Exp, bias=rs[:, 8:9]), reads=["elm", "rs8"], writes=["ex"])
                    P.op("act", lambda: A.activation(out=rs[:, 9:10], in_=top8[:, 1:2], func=AF.Exp, bias=rs[:, 8:9]), reads=["top8", "rs8"], writes=["rs9"])
                    P.op("dve", lambda: V.tensor_scalar(out=rs[:, 10:11], in0=rs[:, 9:10], scalar1=1.0, scalar2=None, op0=ALU.add), reads=["rs9"], writes=["rs10"])
                    P.op("dve", lambda: V.reciprocal(out=rs[:, 11:12], in_=rs[:, 10:11]), reads=["rs10"], writes=["rs11"])
                    P.op("dve", lambda: V.tensor_tensor(out=rs[:, 12:13], in0=rs[:, 11:12], in1=rs[:, 7:8], op=ALU.mult), reads=["rs11", "rs7"], writes=["rs12"])
                    P.op("dve", lambda n=n: V.scalar_tensor_tensor(out=WTs[:, n, :], in0=ex[:], scalar=rs[:, 12:13], in1=sel[:], op0=ALU.mult, op1=ALU.mult),
                         reads=["ex", "sel", "rs12"], writes=["WTs_%d" % n])
                if "wts" in dbg_out:
                    P.dma("sp", dbg_out["wts"].rearrange("(n p) e -> p n e", p=128), WTs[:], reads=["WTs_%d" % n for n in range(NT)], writes=["dbg_wts"])
                for j in range(16):
                    bi = j % 2
                    if j + 1 < 16:
                        load_experts(j + 1)
                    for e in range(2):
                        for fc in range(2):
                            P.op("dve", lambda e=e, fc=fc, bi=bi: V.tensor_tensor(out=WDn[bi][:, e, fc, :], in0=WDn[bi][:, e, fc, :], in1=g2bc[:], op=ALU.mult),
                                 reads=["WDn%d" % bi, "g2bc"], writes=["WDn%d" % bi])
                    def gu(n):
                        np_ = n % 2
                        for e in range(2):
                            pg = PG[np_ * 2 + e]
                            pgk = "PG%d" % (np_ * 2 + e)
                            for kc in range(8):
                                P.op("pe", lambda e=e, kc=kc, pg=pg: T.matmul(pg[:, 0:512], H2T[:, kc, n * 128:(n + 1) * 128], WGU[bi][:, e, kc, :], start=(kc == 0), stop=(kc == 7)),
                                     reads=["H2T_%d" % n, "WGU%d" % bi], writes=[pgk])
                        for e in range(2):
                            pg = PG[np_ * 2 + e]
                            pgk = "PG%d" % (np_ * 2 + e)
                            sg_ = sgm[np_ * 2 + e]
                            hd_ = hid[np_ * 2 + e]
                            P.op("act", lambda pg=pg, sg_=sg_: A.activation(out=sg_[:], in_=pg[:, 0:256], func=AF.Silu), reads=[pgk], writes=["sgm%d" % (np_ * 2 + e)])
                            P.op("dve", lambda e=e, pg=pg, sg_=sg_, hd_=hd_: V.scalar_tensor_tensor(out=hd_[:], in0=sg_[:], scalar=WTs[:, n, 2 * j + e:2 * j + e + 1], in1=pg[:, 256:512], op0=ALU.mult, op1=ALU.mult),
                                 reads=["sgm%d" % (np_ * 2 + e), pgk, "WTs_%d" % n], writes=["hid%d" % (np_ * 2 + e)])

                    def rest(n):
                        np_ = n % 2
                        hb = np_
                        for e in range(2):
                            hd_ = hid[np_ * 2 + e]
                            for fc in range(2):
                                P.op("pe", lambda e=e, fc=fc, hd_=hd_: T.transpose(PX[:, (e * 2 + fc) * 128:(e * 2 + fc + 1) * 128], hd_[:, fc * 128:(fc + 1) * 128], ident_b),
                                     reads=["hid%d" % (np_ * 2 + e), "cstb"], writes=["PX"])
                        P.op("act", lambda: A.copy(out=HIDT[hb][:].rearrange("p e c t -> p (e c) t"), in_=PX[:, 0:512].rearrange("p (c t) -> p c t", c=4)), reads=["PX"], writes=["HIDT%d" % hb])

                    def down(n):
                        np_ = n % 2
                        hb = np_
                        for half in range(2):
                            pd = PD[half]
                            pdk = "PD%d" % half
                            i = 0
                            for e in range(2):
                                for fc in range(2):
                                    P.op("pe", lambda e=e, fc=fc, pd=pd, half=half, i=i: T.matmul(pd[:, 0:512], HIDT[hb][:, e, fc, :], WDn[bi][:, e, fc, half * 512:(half + 1) * 512], start=(i == 0), stop=(i == 3)),
                                         reads=["HIDT%d" % hb, "WDn%d" % bi], writes=[pdk])
                                    i += 1
                            P.op("dve", lambda half=half, pd=pd: V.tensor_tensor(out=HL[:, n, half * 512:(half + 1) * 512], in0=HL[:, n, half * 512:(half + 1) * 512], in1=pd[:, 0:512], op=ALU.add),
                                 reads=[pdk, "HL_%d" % n], writes=["HL_%d" % n])

                    gu(0)
                    rest(0)
                    for n in range(NT):
                        if n + 1 < NT:
                            gu(n + 1)
                        down(n)
                        if n + 1 < NT:
                            rest(n + 1)
                for n in range(NT):
                    P.op("act", lambda n=n: A.activation(out=sqf[:], in_=HL[:, n, :], func=AF.Square, accum_out=rs[:, 0:1]), reads=["HL_%d" % n], writes=["sqf", "rs0"])
                    P.op("act", lambda: A.activation(out=rs[:, 1:2], in_=rs[:, 0:1], func=AF.Sqrt, bias=eps_ap, scale=1.0 / D), reads=["rs0", "cst"], writes=["rs1"])
                    P.op("dve", lambda: V.reciprocal(out=rs[:, 2:3], in_=rs[:, 1:2]), reads=["rs1"], writes=["rs2"])
                    P.op("dve", lambda n=n: V.scalar_tensor_tensor(out=yo[:], in0=HL[:, n, :], scalar=rs[:, 2:3], in1=nfbc[:], op0=ALU.mult, op1=ALU.mult),
                         reads=["HL_%d" % n, "rs2", "nfbc"], writes=["yo"])
                    P.dma("sp", y_d[n * 128:(n + 1) * 128, :], yo[:], reads=["yo"], writes=["y_%d" % n])
                P.wait_all("sp")
                P.wait_all("act")
    return nc, P


_CACHE = {}


def _get_program():
    if "nc" not in _CACHE:
        _, P1 = build_program(None)
        nc, P2 = build_program(P1.needed_out)
        _CACHE["nc"] = nc
    return _CACHE["nc"]


def _host_consts(j):
    c = np.zeros((128, NCST), np.float64)
    idx = np.arange(128)
    s_, t_ = idx[:, None], idx[None, :]
    c[:, C_ID:C_ID + 128] = np.eye(128)
    c[:, C_LE:C_LE + 128] = (s_ <= t_)
    c[:, C_GT:C_GT + 128] = (s_ > t_)
    c[:, C_GE:C_GE + 128] = (s_ >= t_)
    c[:, C_LT:C_LT + 128] = (s_ < t_)
    c[:, C_ONE:C_ONE + 128] = 1.0
    ks = 128.0 ** -0.5
    t = idx.astype(np.float64)
    for h in range(4):
        lf = np.log1p(-2.0 ** (-5.0 - h))
        lb = np.log1p(-2.0 ** (-5.5 - h))
        f = [np.exp(lf * (t + 1)), np.exp(-lf * (t + 1)) * ks, np.exp(lf * (127 - t)) * ks,
             np.exp(lb * (128 - t)), np.exp(-lb * (128 - t)) * ks, np.exp(lb * t) * ks]
        for i in range(6):
            c[:, C_RF + h * 6 + i] = f[i]
        c[:, C_RD + 2 * h] = np.exp(lf * 128)
        c[:, C_RD + 2 * h + 1] = np.exp(lb * 128)
        c[:, C_RDT + 2 * h] = np.exp(lf * SEG)
        c[:, C_RDT + 2 * h + 1] = np.exp(lb * SEG)
    for i in range(4):
        c[:, C_CM + i] = 1.0 if i < j else 0.0
        c[:, C_CM + 4 + i] = 1.0 if i > j else 0.0
    c[:, C_EPS] = EPS
    c[:, C_EPS + 1] = np.log(128.0 ** -0.5)
    inv = (10000.0 ** (-np.arange(32, dtype=np.float32) / np.float32(32))).astype(np.float32)
    for n in range(NT):
        pos = SEG * j + n * 128 + idx
        row = (pos // 64).astype(np.float32)
        col = (pos % 64).astype(np.float32)
        ar = (row[:, None] * inv[None, :]).astype(np.float32).astype(np.float64)
        ac = (col[:, None] * inv[None, :]).astype(np.float32).astype(np.float64)
        c[:, C_CS + n * 256:C_CS + n * 256 + 128] = np.concatenate([np.cos(ar), np.cos(ar), np.cos(ac), np.cos(ac)], axis=1)
        c[:, C_CS + n * 256 + 128:C_CS + (n + 1) * 256] = np.concatenate([-np.sin(ar), np.sin(ar), -np.sin(ac), np.sin(ac)], axis=1)
    return c.astype(np.float32)


def make_in_maps(x, c, ctx, c_ctx, w_ada, b_ada, norm_mix, norm_ffn, w_in, gla_lr_w, gla_lr_b, gla_norm,
                 w_branch_gla, w_branch_ret, w_out, w_router_group, b_router_group, w_router_expert,
                 b_router_expert, w_expert_gate, w_expert_up, w_expert_down, norm_final):
    f = lambda a: np.ascontiguousarray(np.asarray(a, dtype=np.float32))
    x, c, ctx, c_ctx = f(x), f(c), f(ctx), f(c_ctx)
    fm = lambda v: np.ascontiguousarray(v.reshape(-1, 128).T)
    b_ada0 = f(b_ada)[0]
    shared = {
        "w_ada": f(w_ada)[0], "w_in": f(w_in)[0],
        "lrw": np.ascontiguousarray(np.concatenate([f(gla_lr_w)[0].transpose(1, 0, 2), f(gla_lr_b)[0][None]], axis=0)),
        "w_branch_gla": f(w_branch_gla)[0], "w_branch_ret": f(w_branch_ret)[0], "w_out": f(w_out)[0],
        "w_router": np.ascontiguousarray(np.concatenate([f(w_router_group)[0], f(w_router_expert)[0]], axis=1)),
        "b_router": np.ascontiguousarray(np.concatenate([f(b_router_group)[0], f(b_router_expert)[0]])[None, :]),
        "w_expert_gate": f(w_expert_gate)[0], "w_expert_up": f(w_expert_up)[0], "w_expert_down": f(w_expert_down)[0],
    }
    gn_row = np.zeros((D,), np.float32)
    gn_row[:256] = f(gla_norm)[0]
    maps = []
    for i in range(8):
        b, j = i // 4, i % 4
        vec = np.concatenate([fm(c[b]), fm(c_ctx), fm(b_ada0[0:1024]), fm(b_ada0[1024:2048]), fm(b_ada0[3072:4096]),
                              fm(b_ada0[4096:5120]), fm(f(norm_mix)[0]), fm(f(norm_ffn)[0]), fm(f(gla_norm)[0]),
                              np.zeros((128, 6), np.float32)], axis=1)
        rowv = np.stack([b_ada0[2048:3072], b_ada0[5120:6144], f(norm_final), gn_row], axis=0)
        m = dict(shared)
        m.update({"x": np.ascontiguousarray(x[b, j * SEG:(j + 1) * SEG]), "ctx": np.ascontiguousarray(ctx[b]),
                  "cst": _host_consts(j), "vec": np.ascontiguousarray(vec), "rowv": np.ascontiguousarray(rowv)})
        maps.append(m)
    return maps


def kernel(**inputs):
    nc = _get_program()
    maps = make_in_maps(**inputs)
    res = run_bass_kernel_spmd(nc, maps, core_ids=list(range(8)))
    out = np.empty((2, 4 * SEG, D), np.float32)
    for i in range(8):
        out[i // 4, (i % 4) * SEG:(i % 4 + 1) * SEG] = res.results[i]["y"]
    return out
```
